# Optimizing a Trainium2 kernel written in Bass

```python
import jax, jax.numpy as jnp
from jax import lax
import numpy as np

D_MODEL = 1024
BATCH = 2
SEQ = 8192
DEPTH = 1

D_MIX = D_MODEL
D_ATTN = D_MIX // 2
D_LRU = D_MIX - D_ATTN
HEAD_DIM = 64
N_ATTN_HEADS = D_ATTN // HEAD_DIM
N_LRU_BLOCKS = 8
LRU_BLOCK = D_LRU // N_LRU_BLOCKS
CONV_WIDTH = 4
LRU_C = 8.0
Q_BLOCK = 128
N_GROUPS = 4
EXPERTS_PER_GROUP = 4
N_EXPERTS = N_GROUPS * EXPERTS_PER_GROUP
TOP_K_IN_GROUP = 2
D_EXPERT = D_MODEL // 2
LN_EPS = 1e-5
RMS_EPS = 1e-6
NEG_INF = -1e30
DEEPNORM_ALPHA = (2.0 * DEPTH) ** 0.25
DEEPNORM_BETA = (8.0 * DEPTH) ** -0.25
D_IN = 3 * D_ATTN + N_ATTN_HEADS + 2 * D_LRU

kernel_name = "fox_rglru_hmoe_adaln_deepnorm_block"


def layer_norm(x, gain=None, bias=None):
    xf = x.astype(jnp.float32)
    mu = jnp.mean(xf, axis=-1, keepdims=True)
    var = jnp.mean(jnp.square(xf - mu), axis=-1, keepdims=True)
    y = (xf - mu) * lax.rsqrt(var + LN_EPS)
    if gain is not None:
        y = y * gain.astype(jnp.float32) + bias.astype(jnp.float32)
    return y.astype(x.dtype)


def rms_norm(x, gain):
    xf = x.astype(jnp.float32)
    y = xf * lax.rsqrt(jnp.mean(jnp.square(xf), axis=-1, keepdims=True) + RMS_EPS)
    return (y * gain.astype(jnp.float32)).astype(x.dtype)


def forgetting_attention(q, k, v, f_logit):
    b, s, h, dh = q.shape
    cum = jnp.cumsum(jax.nn.log_sigmoid(f_logit.astype(jnp.float32)), axis=1)
    cum = jnp.transpose(cum, (0, 2, 1))
    q = jnp.transpose(q, (0, 2, 1, 3)) * (dh ** -0.5)
    k = jnp.transpose(k, (0, 2, 1, 3))
    v = jnp.transpose(v, (0, 2, 1, 3))
    outs = []
    for i in range(s // Q_BLOCK):
        q0 = i * Q_BLOCK
        kend = q0 + Q_BLOCK
        qb = q[:, :, q0:kend]
        kb = k[:, :, :kend]
        vb = v[:, :, :kend]
        logits = jnp.einsum('bhqd,bhkd->bhqk', qb, kb).astype(jnp.float32)
        logits = logits + (cum[:, :, q0:kend, None] - cum[:, :, None, :kend])
        causal = jnp.arange(kend)[None, :] <= (q0 + jnp.arange(Q_BLOCK))[:, None]
        logits = jnp.where(causal, logits, NEG_INF)
        p = jax.nn.softmax(logits, axis=-1).astype(v.dtype)
        outs.append(jnp.einsum('bhqk,bhkd->bhqd', p, vb))
    o = jnp.concatenate(outs, axis=2)
    return jnp.transpose(o, (0, 2, 1, 3)).reshape(b, s, h * dh)


def rg_lru_branch(xb, gb, conv_w, conv_b, w_rg, b_rg, w_ig, b_ig, lru_lambda):
    b, s, _ = xb.shape
    xc = lax.conv_general_dilated(
        xb, conv_w[:, None, :].astype(xb.dtype), window_strides=(1,),
        padding=((CONV_WIDTH - 1, 0),), dimension_numbers=('NWC', 'WIO', 'NWC'),
        feature_group_count=D_LRU) + conv_b
    xr = xc.reshape(b, s, N_LRU_BLOCKS, LRU_BLOCK)
    r = jax.nn.sigmoid(jnp.einsum('bsni,nij->bsnj', xr, w_rg).reshape(b, s, D_LRU) + b_rg)
    ig = jax.nn.sigmoid(jnp.einsum('bsni,nij->bsnj', xr, w_ig).reshape(b, s, D_LRU) + b_ig)
    log_a = -LRU_C * r.astype(jnp.float32) * jax.nn.softplus(-lru_lambda.astype(jnp.float32))
    a = jnp.exp(log_a)
    u = jnp.sqrt(-jnp.expm1(2.0 * log_a)) * (ig * xc).astype(jnp.float32)

    def combine(e1, e2):
        a1, b1 = e1
        a2, b2 = e2
        return a1 * a2, a2 * b1 + b2

    _, hs = lax.associative_scan(combine, (a, u), axis=1)
    return hs.astype(xb.dtype) * jax.nn.gelu(gb)


def hybrid_mixer(u, w_in, b_f, conv_w, conv_b, w_rg, b_rg, w_ig, b_ig, lru_lambda,
                 g_attn, g_lru, w_out):
    b, s, _ = u.shape
    proj = jnp.einsum('bsd,de->bse', u, w_in)
    q, k, v, f_logit, xb, gb = jnp.split(
        proj, [D_ATTN, 2 * D_ATTN, 3 * D_ATTN, 3 * D_ATTN + N_ATTN_HEADS,
               3 * D_ATTN + N_ATTN_HEADS + D_LRU], axis=-1)
    hs = (b, s, N_ATTN_HEADS, HEAD_DIM)
    attn = forgetting_attention(q.reshape(hs), k.reshape(hs), v.reshape(hs), f_logit + b_f)
    lru = rg_lru_branch(xb, gb, conv_w, conv_b, w_rg, b_rg, w_ig, b_ig, lru_lambda)
    mixed = jnp.concatenate([rms_norm(attn, g_attn), rms_norm(lru, g_lru)], axis=-1)
    return jnp.einsum('bse,ed->bsd', mixed, w_out)


def hierarchical_moe(u, w_grp, b_grp, w_exp, b_exp, w_e_gate, w_e_up, w_e_down):
    b, s, d = u.shape
    t = u.reshape(b * s, d)
    grp_prob = jax.nn.softmax((t @ w_grp + b_grp).astype(jnp.float32), axis=-1)
    grp_w, grp_idx = lax.top_k(grp_prob, 1)
    exp_logits = (t @ w_exp + b_exp).astype(jnp.float32).reshape(-1, N_GROUPS, EXPERTS_PER_GROUP)
    sel_logits = jnp.take_along_axis(exp_logits, grp_idx[:, :, None], axis=1)[:, 0]
    top_w, top_idx = lax.top_k(jax.nn.softmax(sel_logits, axis=-1), TOP_K_IN_GROUP)
    top_w = top_w / jnp.sum(top_w, axis=-1, keepdims=True) * grp_w
    expert_id = grp_idx * EXPERTS_PER_GROUP + top_idx
    comb = jnp.sum(jax.nn.one_hot(expert_id, N_EXPERTS, dtype=jnp.float32)
                   * top_w[..., None], axis=1).astype(t.dtype)
    y = jnp.zeros_like(t)
    for e in range(N_EXPERTS):
        h = jax.nn.silu(t @ w_e_gate[e]) * (t @ w_e_up[e])
        y = y + comb[:, e:e + 1] * (h @ w_e_down[e])
    return y.reshape(b, s, d)


def setup_inputs(seed: int = 0) -> dict:
    key = jax.random.key(seed)
    ks = jax.random.split(key, 28)
    nrm = jax.random.normal
    L = DEPTH
    x = nrm(ks[0], (BATCH, SEQ, D_MODEL), jnp.float32)
    c = nrm(ks[1], (BATCH, D_MODEL), jnp.float32)
    w_ada = nrm(ks[2], (L, D_MODEL, 6 * D_MODEL)) * (0.1 * D_MODEL ** -0.5)
    b_ada = nrm(ks[3], (L, 6 * D_MODEL)) * 0.01
    w_in = nrm(ks[4], (L, D_MODEL, D_IN)) * D_MODEL ** -0.5
    w_in = w_in.at[..., 2 * D_ATTN:3 * D_ATTN].multiply(DEEPNORM_BETA)
    b_f = jax.random.uniform(ks[5], (L, N_ATTN_HEADS), minval=1.0, maxval=6.0)
    conv_w = nrm(ks[6], (L, CONV_WIDTH, D_LRU)) * CONV_WIDTH ** -0.5
    conv_b = nrm(ks[7], (L, D_LRU)) * 0.01
    w_rg = nrm(ks[8], (L, N_LRU_BLOCKS, LRU_BLOCK, LRU_BLOCK)) * LRU_BLOCK ** -0.5
    b_rg = nrm(ks[9], (L, D_LRU)) * 0.01
    w_ig = nrm(ks[10], (L, N_LRU_BLOCKS, LRU_BLOCK, LRU_BLOCK)) * LRU_BLOCK ** -0.5
    b_ig = nrm(ks[11], (L, D_LRU)) * 0.01
    a_c = jax.random.uniform(ks[12], (L, D_LRU), minval=0.9, maxval=0.999)
    a0 = a_c ** (1.0 / LRU_C)
    lru_lambda = jnp.log(a0) - jnp.log1p(-a0)
    g_attn = 1.0 + 0.02 * nrm(ks[13], (L, D_ATTN))
    g_lru = 1.0 + 0.02 * nrm(ks[14], (L, D_LRU))
    w_out = nrm(ks[15], (L, D_MIX, D_MODEL)) * (D_MIX ** -0.5 * DEEPNORM_BETA)
    ln1_g = 1.0 + 0.02 * nrm(ks[16], (L, D_MODEL))
    ln1_b = 0.01 * nrm(ks[17], (L, D_MODEL))
    w_grp = nrm(ks[18], (L, D_MODEL, N_GROUPS)) * D_MODEL ** -0.5
    b_grp = 0.01 * nrm(ks[19], (L, N_GROUPS))
    w_exp = nrm(ks[20], (L, D_MODEL, N_EXPERTS)) * D_MODEL ** -0.5
    b_exp = 0.01 * nrm(ks[21], (L, N_EXPERTS))
    w_e_gate = nrm(ks[22], (L, N_EXPERTS, D_MODEL, D_EXPERT)) * D_MODEL ** -0.5
    w_e_up = nrm(ks[23], (L, N_EXPERTS, D_MODEL, D_EXPERT)) * D_MODEL ** -0.5
    w_e_down = nrm(ks[24], (L, N_EXPERTS, D_EXPERT, D_MODEL)) * (D_EXPERT ** -0.5 * DEEPNORM_BETA)
    ln2_g = 1.0 + 0.02 * nrm(ks[25], (L, D_MODEL))
    ln2_b = 0.01 * nrm(ks[26], (L, D_MODEL))
    return {"x": x, "c": c, "w_ada": w_ada, "b_ada": b_ada, "w_in": w_in, "b_f": b_f,
            "conv_w": conv_w, "conv_b": conv_b, "w_rg": w_rg, "b_rg": b_rg,
            "w_ig": w_ig, "b_ig": b_ig, "lru_lambda": lru_lambda, "g_attn": g_attn,
            "g_lru": g_lru, "w_out": w_out, "ln1_g": ln1_g, "ln1_b": ln1_b,
            "w_grp": w_grp, "b_grp": b_grp, "w_exp": w_exp, "b_exp": b_exp,
            "w_e_gate": w_e_gate, "w_e_up": w_e_up, "w_e_down": w_e_down,
            "ln2_g": ln2_g, "ln2_b": ln2_b}


def reference(x, c, w_ada, b_ada, w_in, b_f, conv_w, conv_b, w_rg, b_rg, w_ig, b_ig,
              lru_lambda, g_attn, g_lru, w_out, ln1_g, ln1_b, w_grp, b_grp, w_exp, b_exp,
              w_e_gate, w_e_up, w_e_down, ln2_g, ln2_b):
    for l in range(DEPTH):
        mod = jnp.einsum('bd,de->be', jax.nn.silu(c), w_ada[l]) + b_ada[l]
        sh_m, sc_m, gt_m, sh_f, sc_f, gt_f = jnp.split(mod[:, None, :], 6, axis=-1)
        u = layer_norm(x) * (1.0 + sc_m) + sh_m
        mix = hybrid_mixer(u, w_in[l], b_f[l], conv_w[l], conv_b[l], w_rg[l], b_rg[l],
                           w_ig[l], b_ig[l], lru_lambda[l], g_attn[l], g_lru[l], w_out[l])
        x = layer_norm(DEEPNORM_ALPHA * x + (1.0 + gt_m) * mix, ln1_g[l], ln1_b[l])
        u = layer_norm(x) * (1.0 + sc_f) + sh_f
        ffn = hierarchical_moe(u, w_grp[l], b_grp[l], w_exp[l], b_exp[l],
                               w_e_gate[l], w_e_up[l], w_e_down[l])
        x = layer_norm(DEEPNORM_ALPHA * x + (1.0 + gt_f) * ffn, ln2_g[l], ln2_b[l])
    return x
```

```python
import numpy as np
import ml_dtypes
import concourse.bass as bass
import concourse.mybir as mybir
from concourse.bass_utils import run_bass_kernel_spmd

F32 = mybir.dt.float32
BF16 = mybir.dt.bfloat16
U8 = mybir.dt.uint8
I32 = mybir.dt.int32
TT = 256
NT = 31
NSLOT = NT * TT
NT_RUN = [NT]
ALU = mybir.AluOpType
AF = mybir.ActivationFunctionType
AX = mybir.AxisListType

NEG = -30000.0
ALPHA = 2.0 ** 0.25


class Dep:
    __slots__ = ("name", "writers", "readers")

    def __init__(self, name=""):
        self.name = name
        self.writers = []
        self.readers = []


class _Op:
    __slots__ = ("idx", "eng", "fn", "deps", "dma", "key", "need_inc", "tok", "ndma", "inc")


COMPUTE = ("pe", "act", "dve", "pool")
STREAMS = ("pe", "act", "dve", "pool", "sp")


class Prog:
    def __init__(self):
        self.ops = []
        self.streams = {e: [] for e in STREAMS}
        self.dma_counts = {}
        self.last_compute = {e: None for e in COMPUTE}
        self.dma_open = []
        self.fence_set = set()
        self.keymap = {}
        self.free_keys = {True: [], False: []}
        self.key_sw = {}
        self.pinned = set()

    def op(self, eng, fn, R=(), W=(), Wp=(), dma=False, key=None, ndma=1, inc=16, nofence=False):
        o = _Op()
        o.inc = inc
        o.idx = len(self.ops)
        o.eng = eng
        o.fn = fn
        o.dma = dma
        o.key = key
        o.ndma = ndma
        o.need_inc = False
        o.tok = None
        deps = set(self.fence_set)
        raw = set()
        for r in R:
            deps.update(r.writers)
            raw.update(r.writers)
        for w in list(W) + list(Wp):
            deps.update(w.writers)
            deps.update(w.readers)
        keep = set()
        for d in deps:
            p = self.ops[d]
            if dma or p.dma or p.eng != eng:
                keep.add(d)
            elif eng != "pe":
                keep.add(d)
        o.deps = keep
        for d in keep:
            self.ops[d].need_inc = True
        for r in R:
            r.readers.append(o.idx)
        for w in W:
            w.writers = [o.idx]
            w.readers = []
        for w in Wp:
            w.writers = w.writers + [o.idx]
            w.readers = []
        if dma:
            pk = self.keymap.get(key)
            if pk is None:
                sw = (eng == "pool")
                fk = self.free_keys[sw]
                pk = fk.pop() if (fk and not nofence and inc == 16) else "k%d_%s" % (len(self.dma_counts), key)
                self.key_sw[pk] = sw
                self.keymap[key] = pk
            if nofence or inc != 16:
                self.pinned.add(pk)
            key = pk
            o.key = pk
            c = self.dma_counts.get(key, 0) + inc * ndma
            self.dma_counts[key] = c
            o.tok = (key, c)
            if not nofence:
                self.dma_open.append(o.idx)
        else:
            self.last_compute[eng] = o.idx
        self.ops.append(o)
        self.streams[eng].append(o)
        return o

    def fence(self):
        s = set(self.dma_open)
        for e in COMPUTE:
            if self.last_compute[e] is not None:
                s.add(self.last_compute[e])
        self.fence_set = s
        self.dma_open = []
        for k, pk in list(self.keymap.items()):
            if pk not in self.pinned:
                self.free_keys[self.key_sw[pk]].append(pk)
                del self.keymap[k]

    def finalize(self):
        cnt = {e: 0 for e in COMPUTE}
        for o in self.ops:
            if o.dma:
                continue
            if o.need_inc:
                cnt[o.eng] += 1
                o.tok = ("eng_" + o.eng, cnt[o.eng])
        self.eng_counts = cnt

    def emit(self, block, sems, final_waits=()):
        prog = self
        engs = {"pe": "tensor", "act": "scalar", "dve": "vector", "pool": "gpsimd", "sp": "sync"}

        def make(stream):
            def body(e):
                seen = {}
                for o in prog.streams[stream]:
                    need = {}
                    for d in o.deps:
                        k, v = prog.ops[d].tok
                        if need.get(k, 0) < v:
                            need[k] = v
                    for k, v in need.items():
                        if seen.get(k, 0) >= v:
                            continue
                        e.wait_ge(sems[k], v)
                        seen[k] = v
                    res = o.fn(e)
                    if not isinstance(res, (list, tuple)):
                        res = [res]
                    if o.dma:
                        assert len(res) == o.ndma, (len(res), o.ndma)
                        for ins in res:
                            ins.then_inc(sems[o.key], o.inc)
                    elif o.need_inc:
                        res[-1].then_inc(sems[o.tok[0]], 1)
                if stream == "sp":
                    for k, v in final_waits:
                        e.wait_ge(sems[k], v)
            return body

        for stream in STREAMS:
            getattr(block, engs[stream])(make(stream))


class Arena:
    def __init__(self, ap, size):
        self.ap = ap
        self.size = size
        self.top = 0

    def take(self, shape, dt, parts=None):
        esz = {F32: 4, BF16: 2, U8: 1, I32: 4}[dt]
        n = 1
        for s in shape[1:]:
            n *= s
        nb = n * esz
        off = (self.top + 63) // 64 * 64
        assert off + nb <= self.size, ("arena overflow", off, nb, self.size)
        self.top = off + nb
        v = self.ap[0:shape[0], off:off + nb]
        if dt != U8:
            v = v.bitcast(dt)
        if len(shape) == 3:
            v = v.rearrange("p (a b) -> p a b", a=shape[1])
        elif len(shape) == 4:
            v = v.rearrange("p (a b c) -> p a b c", a=shape[1], b=shape[2])
        return v


def build(stage=99):
    nc = bass.Bass("TRN2", target_bir_lowering=False)

    def din(name, shape, dt=F32):
        return nc.dram_tensor(name, shape, dt, kind="ExternalInput").ap()

    x_loc = din("x_loc", [2048, 1024])
    xh_in = din("xh", [48, 1024])
    c_in = din("c", [1024])
    oh_in = din("oh", [128, 4])
    hv_in = din("hv", [128, 48])
    mask_in = din("maskT", [128, 4, 128], BF16)
    w_ada_sh = din("w_ada_sh", [1024, 1536])
    b_ada_sh = din("b_ada_sh", [1536])
    w_in = din("w_in", [1024, 2568])
    b_f = din("b_f", [8])
    conv_w = din("conv_w", [4, 512])
    conv_b = din("conv_b", [512])
    w_rg = din("w_rg", [8, 64, 64])
    b_rg = din("b_rg", [512])
    w_ig = din("w_ig", [8, 64, 64])
    b_ig = din("b_ig", [512])
    lru_lambda = din("lru_lambda", [512])
    g_attn = din("g_attn", [512])
    g_lru = din("g_lru", [512])
    w_out = din("w_out", [1024, 1024])
    ln1_g = din("ln1_g", [1024])
    ln1_b = din("ln1_b", [1024])
    w_grp = din("w_grp", [1024, 4])
    b_grp = din("b_grp", [4])
    w_exp = din("w_exp", [1024, 16])
    b_exp = din("b_exp", [16])
    wsh_in = [din(n, [4 * 128, 4096]) for n in ("wg_sh", "wu_sh", "wd_sh")]
    Wb_loc = [nc.dram_tensor(f"Wb_loc{m}", [4 * 128, 4096], BF16).ap() for m in range(3)]
    Wb_all = [nc.dram_tensor(f"Wb_all{m}", [16 * 128, 4096], BF16).ap() for m in range(3)]
    cstb_in = din("cstb", [128, 256], BF16)
    cstf_in = din("cstf", [128, 80])
    ln2_g = din("ln2_g", [1024])
    ln2_b = din("ln2_b", [1024])
    out = nc.dram_tensor("out", [2048, 1024], F32, kind="ExternalOutput").ap()
    dbg = {}

    def dout(name, shape, dt=F32):
        dbg[name] = nc.dram_tensor(name, shape, dt, kind="ExternalOutput").ap()
        return dbg[name]

    Kd_loc = [nc.dram_tensor(f"Kd_loc{g}", [128, 2048], BF16).ap() for g in range(4)]
    Kd_all = [nc.dram_tensor(f"Kd_all{g}", [512, 2048], BF16).ap() for g in range(4)]
    Vd_loc = [nc.dram_tensor(f"Vd_loc{g}", [2048, 130], BF16).ap() for g in range(4)]
    Vd_all = [nc.dram_tensor(f"Vd_all{g}", [8192, 130], BF16).ap() for g in range(4)]
    Lf_loc = nc.dram_tensor("Lf_loc", [8, 2048], F32).ap()
    Lf_all = nc.dram_tensor("Lf_all", [32, 2048], F32).ap()
    Bd_loc = nc.dram_tensor("Bd_loc", [128, 128], F32).ap()
    Bd_all = nc.dram_tensor("Bd_all", [512, 128], F32).ap()
    Qd = nc.dram_tensor("Qd", [512, 2048], BF16).ap()
    Cq = nc.dram_tensor("Cq", [8, 3, 2048], BF16).ap()
    RG = [[0, 1, 2, 3], [4, 5, 6, 7]]

    P = Prog()
    ARENA_SZ = 204 * 1024
    arena_t = nc.alloc_sbuf_tensor("arena", [128, ARENA_SZ], U8)
    A = Arena(arena_t, ARENA_SZ)
    banks = [nc.alloc_psum_tensor(f"bank{i}", [128, 512], F32) for i in range(8)]

    def bank_bf(i):
        return banks[i][:, :].bitcast(BF16).rearrange("p (a b) -> p a b", a=8)

    def OP(eng, R, W, *fns, Wp=()):
        return P.op(eng, (lambda e: [f(e) for f in fns]), R=R, W=W, Wp=Wp)

    def CHAIN(eng, R, W, *fns):
        D_ch = Dep("chain")
        for i, f in enumerate(fns):
            P.op(eng, (lambda f=f: lambda e: [f(e)])(), R=list(R) + ([D_ch] if i else []), W=list(W) + [D_ch])

    def DMA(q, R, W, key, *fns, Wp=(), inc=16, nofence=False):
        return P.op(q, (lambda e: [f(e) for f in fns]), R=R, W=W, Wp=Wp, dma=True, key=key, ndma=len(fns), inc=inc, nofence=nofence)

    def dma(out_, in_, **kw):
        return lambda e: e.dma_start(out=out_, in_=in_, **kw)

    def dma_nc(out_, in_):
        return lambda e: e.dma_start(out=out_, in_=in_, allow_slow_non_contiguous=True)

    def mm(o, l, r, st, sp):
        return lambda e: e.matmul(o, lhsT=l, rhs=r, start=st, stop=sp)

    ident_bf = A.take([128, 128], BF16)
    ident_f = A.take([128, 128], F32)
    ones_f = A.take([128, 128], F32)
    modT = A.take([128, 32], F32)
    gtm_bc = A.take([128, 1024], F32)
    gtf_bc = A.take([128, 1024], F32)
    oh = A.take([128, 4], F32)
    D_const = Dep("const")
    D_mod = Dep("mod")
    D_gtm = Dep("gtm")
    D_gtf = Dep("gtf")
    D_oh = Dep("oh")

    def mk_ident(t):
        return [lambda e: e.memset(t, 1.0),
                lambda e: e.affine_select(out=t, in_=t, pattern=[[-1, 128]], compare_op=ALU.is_equal,
                                          fill=0.0, base=0, channel_multiplier=1)]
    CHAIN("pool", [], [D_const], *mk_ident(ident_bf[:]))
    CHAIN("pool", [], [Dep("idf")], *mk_ident(ident_f[:]))
    OP("pool", [], [Dep("onesf")], lambda e: e.memset(ones_f[:], 1.0))
    DMA("sp", [], [D_oh], "c_oh", dma(oh[:], oh_in))

    PERSIST = A.top

    ln_ctr = [0]

    def ln_apply(src, npart, D_src, dst, D_dst, scratch, eps=1e-5):
        st, mv, sd, rstd, D_s = scratch
        OP("dve", [D_src], [D_s],
           lambda e: e.bn_stats(out=st[0:npart, 0, :], in_=src[:, 0:512]),
           lambda e: e.bn_stats(out=st[0:npart, 1, :], in_=src[:, 512:1024]))
        OP("dve", [D_s], [D_s], lambda e: e.bn_aggr(out=mv[0:npart, :], in_=st[0:npart].rearrange("p a b -> p (a b)")))
        OP("act", [D_s], [D_s], lambda e: e.activation(out=sd[0:npart, :], in_=mv[0:npart, 1:2], func=AF.Sqrt, bias=eps, scale=1.0))
        OP("dve", [D_s], [D_s], lambda e: e.reciprocal(out=rstd[0:npart, :], in_=sd[0:npart, :]))
        OP("dve", [D_src, D_s], [D_dst],
           lambda e: e.tensor_scalar(out=dst, in0=src, scalar1=mv[0:npart, 0:1], scalar2=rstd[0:npart, 0:1],
                                     op0=ALU.subtract, op1=ALU.mult))

    def ln_apply_gen(src, npart, D_src, dst, D_dst, scratch, eps=1e-5, nmr=None):
        st, mv, sd, rstd, D_s = scratch
        OP("dve", [D_src], [D_s],
           lambda e: e.bn_stats(out=st[0:npart, 0, :], in_=src[:, 0:512]),
           lambda e: e.bn_stats(out=st[0:npart, 1, :], in_=src[:, 512:1024]))
        yield
        OP("dve", [D_s], [D_s], lambda e: e.bn_aggr(out=mv[0:npart, :], in_=st[0:npart].rearrange("p a b -> p (a b)")))
        yield
        OP("act", [D_s], [D_s], lambda e: e.activation(out=sd[0:npart, :], in_=mv[0:npart, 1:2], func=AF.Sqrt, bias=eps, scale=1.0))
        yield
        OP("dve", [D_s], [D_s], lambda e: e.reciprocal(out=rstd[0:npart, :], in_=sd[0:npart, :]))
        yield
        if nmr is None:
            OP("dve", [D_src, D_s], [D_dst],
               lambda e: e.tensor_scalar(out=dst, in0=src, scalar1=mv[0:npart, 0:1], scalar2=rstd[0:npart, 0:1],
                                         op0=ALU.subtract, op1=ALU.mult))
        else:
            OP("dve", [D_s], [D_s], lambda e: e.scalar_tensor_tensor(
                out=nmr[0:npart, :], in0=mv[0:npart, 0:1], scalar=-1.0, in1=rstd[0:npart, :], op0=ALU.mult, op1=ALU.mult))
            yield
            OP("act", [D_src, D_s], [D_dst], lambda e: e.activation(
                out=dst, in_=src, func=AF.Identity, scale=rstd[0:npart, 0:1], bias=nmr[0:npart, 0:1]))
        yield

    def mk_scratch_in(AA):
        return (AA.take([128, 2, 6], F32), AA.take([128, 2], F32), AA.take([128, 1], F32), AA.take([128, 1], F32), Dep("lnsc"))

    def mk_scratch():
        return (A.take([128, 2, 6], F32), A.take([128, 2], F32), A.take([128, 1], F32), A.take([128, 1], F32), Dep("lnsc"))

    m0 = A.top
    cT = A.take([128, 8], F32)
    csil = A.take([128, 8], F32)
    waf = A.take([128, 8, 1536], F32)
    brow = A.take([1, 1536], F32)
    mrow = A.take([1, 1536], F32)
    D_c = Dep("c")
    D_waf = Dep("waf")
    D_brow = Dep("brow")
    D_mrow = Dep("mrow")
    D_pm = [Dep(f"pm{i}") for i in range(3)]
    D_Md = Dep("Md")
    D_Ma = Dep("Ma")
    Md_loc = nc.dram_tensor("Md_loc", [1, 1536], F32).ap()
    Md_all = nc.dram_tensor("Md_all", [4, 1536], F32).ap()
    DMA("sp", [], [D_c], "c_c", dma_nc(cT[:], c_in.rearrange("(k p) -> p k", p=128)))
    DMA("sp", [], [D_brow], "c_bT", dma(brow[:], b_ada_sh.rearrange("(o n) -> o n", o=1)))
    wsh_v = w_ada_sh.rearrange("(k p) e -> p k e", p=128)
    DMA("sp", [], [D_waf], "ld_waf", *[dma(waf[:, 2 * i:2 * i + 2, :], wsh_v[:, 2 * i:2 * i + 2, :]) for i in range(4)])
    OP("act", [D_c], [D_c], lambda e: e.activation(out=csil[:], in_=cT[:], func=AF.Silu))
    for i in range(3):
        pb = banks[i]
        cs_ = slice(i * 512, (i + 1) * 512)
        OP("pe", [D_waf, D_c], [D_pm[i]], *[mm(pb[0:1, :], csil[:, k:k + 1], waf[:, k, cs_], k == 0, k == 7) for k in range(8)])
        OP("dve", [D_pm[i], D_brow], [], (lambda pb=pb, cs_=cs_: lambda e: e.tensor_tensor(
            out=mrow[:, cs_], in0=pb[0:1, :], in1=brow[:, cs_], op=ALU.add))(), Wp=[D_mrow])
    DMA("sp", [D_mrow], [D_Md], "st_md", dma(Md_loc, mrow[:]))
    DMA("pool", [D_Md], [D_Ma], "cc_md", lambda e: e.collective_compute(
        "AllGather", ALU.bypass, replica_groups=RG, ins=[Md_loc], outs=[Md_all]), inc=1)
    mflat = Md_all.rearrange("r c -> (r c)")
    DMA("sp", [D_Ma], [D_mod], "ld_mod",
        *[dma_nc(modT[:, gi * 8:(gi + 1) * 8], mflat[g * 1024:(g + 1) * 1024].rearrange("(k p) -> p k", p=128))
          for gi, g in enumerate([0, 1, 3, 4])])
    DMA("sp", [D_Ma], [D_gtm], "ld_gtm", dma(gtm_bc[:], mflat[2048:3072].partition_broadcast(128)))
    DMA("sp", [D_Ma], [D_gtf], "ld_gtf", dma(gtf_bc[:], mflat[5120:6144].partition_broadcast(128)))
    OP("dve", [D_mod], [D_mod],
       lambda e: e.tensor_scalar(out=modT[:, 8:16], in0=modT[:, 8:16], scalar1=1.0, scalar2=None, op0=ALU.add),
       lambda e: e.tensor_scalar(out=modT[:, 24:32], in0=modT[:, 24:32], scalar1=1.0, scalar2=None, op0=ALU.add))
    OP("pool", [D_gtm], [D_gtm], lambda e: e.tensor_scalar(out=gtm_bc[:], in0=gtm_bc[:], scalar1=1.0, scalar2=None, op0=ALU.add))
    OP("pool", [D_gtf], [D_gtf], lambda e: e.tensor_scalar(out=gtf_bc[:], in0=gtf_bc[:], scalar1=1.0, scalar2=None, op0=ALU.add))
    sh_m, sc_m1, sh_f, sc_f1 = modT[:, 0:8], modT[:, 8:16], modT[:, 16:24], modT[:, 24:32]
    P.fence()
    A.top = m0

    m1 = A.top
    xbh = A.take([128, 4, 16, 131], F32)
    ggb = A.take([128, 4, 2048], BF16)
    hv = A.take([128, 48], F32)
    lfT = A.take([8, 2048], F32)
    m1b = A.top
    uT = A.take([128, 8, 2048], BF16)
    uTh = A.take([128, 8, 48], BF16)
    win = A.take([128, 8, 2568], BF16)
    Vp = A.take([128, 16, 8, 65], BF16)
    qk_st = [A.take([128, 8, 512], BF16) for _ in range(2)]
    xt = [A.take([128, 1024], F32) for _ in range(2)]
    xn = [A.take([128, 1024], BF16) for _ in range(2)]
    lnsc = [mk_scratch() for _ in range(2)]
    gtmp = [A.take([128, 512], F32) for _ in range(3)]
    ftmp = A.take([8, 512], F32)
    bfT = A.take([8, 1], F32)
    D_uT = [Dep(f"uT{b}") for b in range(16)]
    D_uTh = Dep("uTh")
    D_xt = [Dep("xt0"), Dep("xt1")]
    D_xn = [Dep("xn0"), Dep("xn1")]
    D_pT = [Dep("pT0"), Dep("pT1")]
    D_win = [Dep(f"win{i}") for i in range(5)]
    D_xbh = Dep("xbh")
    D_ggb = Dep("ggb")
    D_lfT = Dep("lfT")
    D_Vp = Dep("Vp")
    D_hv = Dep("hv")
    D_bf = Dep("bf")
    win_v = w_in.rearrange("(k p) e -> p k e", p=128)
    colgrp = [(0, 512), (512, 1024), (1024, 1544), (1544, 2056), (2056, 2568)]
    for i, (a, b) in enumerate(colgrp):
        DMA("pool", [], [D_win[i]], f"win{i}", dma(win[:, :, a:b], win_v[:, :, a:b]))
    DMA("sp", [], [D_hv], "c_hv", dma(hv[:], hv_in))
    DMA("sp", [], [D_bf], "c_bf", dma_nc(bfT[:], b_f.rearrange("(h o) -> h o", o=1)))
    OP("pool", [], [D_Vp], lambda e: e.memset(Vp[:, :, :, 64:65], 1.0))
    wst = A.take([128, 4096], BF16)
    D_wst = Dep("wst")
    D_Wbl = [Dep(f"Wbl{m}") for m in range(3)]
    D_Wall = [Dep(f"Wall{m}") for m in range(3)]
    for m in range(3):
        for jj in range(4):
            DMA("pool", [], [D_wst], "ld_wst", dma(wst[:], wsh_in[m][jj * 128:(jj + 1) * 128, :]))
            DMA("pool", [D_wst], [], "st_wst", dma(Wb_loc[m][jj * 128:(jj + 1) * 128, :], wst[:]), Wp=[D_Wbl[m]])

    def ln_to_uT(blk, npart, src_dram, dstT, D_dst, s):
        DMA("sp", [], [D_xt[s]], f"xt{s}", dma(xt[s][0:npart, :], src_dram))
        ln_apply(xt[s][0:npart, :], npart, D_xt[s], xn[s][0:npart, :], D_xn[s], lnsc[s])
        pT = bank_bf(s)
        OP("pe", [D_xn[s], D_const], [D_pT[s]],
           *[(lambda k=k: lambda e: e.transpose(out=pT[:, k, 0:npart], in_=xn[s][0:npart, k * 128:(k + 1) * 128],
                                                identity=ident_bf[0:npart, 0:npart]))() for k in range(8)])
        OP("act", [D_pT[s], D_mod], [], *[(lambda k=k: lambda e: e.activation(
            out=dstT(k), in_=pT[:, k, 0:npart], func=AF.Identity, scale=sc_m1[:, k:k + 1], bias=sh_m[:, k:k + 1]))()
            for k in range(0, 8, 2)], Wp=[D_dst])
        OP("dve", [D_pT[s], D_mod], [], *[(lambda k=k: lambda e: e.tensor_scalar(
            out=dstT(k), in0=pT[:, k, 0:npart], scalar1=sc_m1[:, k:k + 1], scalar2=sh_m[:, k:k + 1],
            op0=ALU.mult, op1=ALU.add))() for k in range(1, 8, 2)], Wp=[D_dst])

    def ln_block(blk):
        ln_to_uT(blk, 128, x_loc[blk * 128:(blk + 1) * 128, :],
                 (lambda blk=blk: lambda k: uT[:, k, blk * 128:(blk + 1) * 128])(), D_uT[blk], blk % 2)

    ln_to_uT(-1, 48, xh_in, lambda k: uTh[:, k, :], D_uTh, 1)
    for blk in range(4):
        ln_block(blk)

    D_pA = [Dep(f"pA{i}") for i in range(4)]
    D_qk = [Dep("qk0"), Dep("qk1")]
    D_gt = [Dep(f"gtmp{i}") for i in range(3)]
    D_pF = Dep("pF")
    D_ft = Dep("ftmp")
    D_Kd = Dep("Kd")
    D_Qd = Dep("Qd")
    pa_i = [0]

    def next_pa():
        i = pa_i[0] % 4
        pa_i[0] += 1
        return banks[2 + i], D_pA[i]

    for cch in range(4):
        pb, D_p = next_pa()
        c0 = 1544 + cch * 128
        OP("pe", [D_uTh, D_win[3]], [D_p], *[mm(pb[:, 0:48], win[:, k, c0:c0 + 128], uTh[:, k, :], k == 0, k == 7) for k in range(8)])
        OP("dve", [D_p, D_hv], [], (lambda pb=pb, cch=cch: lambda e: e.tensor_tensor(
            out=xbh[:, cch, :, 0:3], in0=pb[:, 0:48].rearrange("p (m w) -> p m w", w=3),
            in1=hv[:].rearrange("p (m w) -> p m w", w=3), op=ALU.mult))(), Wp=[D_xbh])

    def do_tile(T):
        tcol = slice(T * 512, (T + 1) * 512)
        R_u = [D_uT[T * 4 + i] for i in range(4)]
        s = T % 2
        for oc in range(8):
            pb, D_p = next_pa()
            c0 = oc * 128
            OP("pe", R_u + [D_win[oc // 4]], [D_p], *[mm(pb[:, :], win[:, k, c0:c0 + 128], uT[:, k, tcol], k == 0, k == 7) for k in range(8)])
            if oc < 4:
                OP("act", [D_p], [], (lambda pb=pb, oc=oc: lambda e: e.activation(out=qk_st[s][:, oc, :], in_=pb[:, :], func=AF.Copy, scale=0.125))(), Wp=[D_qk[s]])
            else:
                OP("dve", [D_p], [], (lambda pb=pb, oc=oc: lambda e: e.tensor_copy(out=qk_st[s][:, oc, :], in_=pb[:, :]))(), Wp=[D_qk[s]])
            yield
        DMA("sp", [D_qk[s]], [], f"qk{s}",
            dma(Qd[:, tcol].rearrange("(c p) t -> p c t", p=128), qk_st[s][:, 0:4, :]),
            *[dma(Kd_loc[g][:, tcol], qk_st[s][:, 4 + g, :]) for g in range(4)], Wp=[D_Kd, D_Qd])
        for cch in range(4):
            pb, D_p = next_pa()
            c0 = 1544 + cch * 128
            OP("pe", R_u + [D_win[3]], [D_p], *[mm(pb[:, :], win[:, k, c0:c0 + 128], uT[:, k, tcol], k == 0, k == 7) for k in range(8)])
            OP("act", [D_p], [], (lambda pb=pb, cch=cch: lambda e: e.activation(
                out=xbh[:, cch, T * 4:(T + 1) * 4, 3:131], in_=pb[:, :].rearrange("p (m w) -> p m w", w=128), func=AF.Copy))(), Wp=[D_xbh])
            yield
        for cch in range(4):
            pb, D_p = next_pa()
            c0 = 2056 + cch * 128
            OP("pe", R_u + [D_win[4]], [D_p], *[mm(pb[:, :], win[:, k, c0:c0 + 128], uT[:, k, tcol], k == 0, k == 7) for k in range(8)])
            g0, g1, g2 = gtmp
            OP("act", [D_p], [D_gt[0]], (lambda pb=pb: lambda e: e.activation(out=g0[:], in_=pb[:, :], func=AF.Copy))())
            CHAIN("dve", [D_gt[0]], [D_gt[1]],
                  lambda e: e.tensor_tensor(out=g1[:], in0=g0[:], in1=g0[:], op=ALU.mult),
                  lambda e: e.tensor_scalar(out=g1[:], in0=g1[:], scalar1=0.044715, scalar2=1.0, op0=ALU.mult, op1=ALU.add),
                  lambda e: e.tensor_tensor(out=g1[:], in0=g1[:], in1=g0[:], op=ALU.mult))
            OP("act", [D_gt[1]], [D_gt[2]], lambda e: e.activation(out=g2[:], in_=g1[:], func=AF.Sigmoid, scale=1.5957691216057308))
            OP("dve", [D_gt[0], D_gt[2]], [], (lambda cch=cch: lambda e: e.tensor_tensor(
                out=ggb[:, cch, tcol], in0=g0[:], in1=g2[:], op=ALU.mult))(), Wp=[D_ggb])
            yield
        pf = banks[6]
        OP("pe", R_u + [D_win[2]], [D_pF], *[mm(pf[0:8, :], win[:, k, 1536:1544], uT[:, k, tcol], k == 0, k == 7) for k in range(8)])
        OP("dve", [D_pF, D_bf], [D_ft], lambda e: e.tensor_scalar(out=ftmp[:], in0=pf[0:8, :], scalar1=bfT[:, 0:1], scalar2=-1.0, op0=ALU.add, op1=ALU.mult))
        OP("act", [D_ft], [D_ft], lambda e: e.activation(out=ftmp[:], in_=ftmp[:], func=AF.Exp))
        OP("act", [D_ft], [], (lambda tcol=tcol: lambda e: e.activation(out=lfT[:, tcol], in_=ftmp[:], func=AF.Ln, bias=1.0, scale=1.0))(), Wp=[D_lfT])
        yield
        for i in range(4):
            blk = T * 4 + i
            pb, D_p = next_pa()
            OP("pe", [D_uT[blk], D_win[2]], [D_p], *[mm(pb[:, :], uT[:, k, blk * 128:(blk + 1) * 128], win[:, k, 1024:1536], k == 0, k == 7) for k in range(8)])
            if i % 2:
                OP("dve", [D_p], [], (lambda pb=pb, blk=blk: lambda e: e.tensor_copy(
                    out=Vp[:, blk, :, 0:64], in_=pb[:, :].rearrange("p (h d) -> p h d", d=64)))(), Wp=[D_Vp])
            else:
                OP("act", [D_p], [], (lambda pb=pb, blk=blk: lambda e: e.activation(
                    out=Vp[:, blk, :, 0:64], in_=pb[:, :].rearrange("p (h d) -> p h d", d=64), func=AF.Copy))(), Wp=[D_Vp])
            yield

    for T in range(4):
        nxt = list(range(4 * (T + 1), 4 * (T + 2))) if T < 3 else []
        for step, _ in enumerate(do_tile(T)):
            if nxt and step % 5 == 4:
                ln_block(nxt.pop(0))
        while nxt:
            ln_block(nxt.pop(0))

    D_Vd = Dep("Vd")
    D_Lfd = Dep("Lfd")
    DMA("sp", [D_Vp], [D_Vd], "st_v", *[dma(Vd_loc[g].rearrange("(m p) f -> p m f", p=128),
                                            Vp[:, :, 2 * g:2 * g + 2, :].rearrange("p m h d -> p m (h d)")) for g in range(4)])
    DMA("sp", [D_lfT], [D_Lfd], "st_lf", dma(Lf_loc, lfT[:]))
    D_Kall = [Dep(f"Kall{g}") for g in range(4)]
    D_Vall = [Dep(f"Vall{g}") for g in range(4)]
    D_Lfall = Dep("Lfall")

    def cc(in_ap, out_ap):
        return lambda e: e.collective_compute("AllGather", ALU.bypass, replica_groups=RG, ins=[in_ap], outs=[out_ap])
    if stage == 0:
        o_q = dout("o_q", [512, 2048], BF16)
        o_xbh = dout("o_xbh", [128, 4 * 16 * 131])
        DMA("sp", [D_Qd], [], "dbg", dma(o_q, Qd))
        DMA("sp", [D_xbh], [], "dbg", dma(o_xbh, xbh[:].rearrange("p a b c -> p (a b c)")))
        o_mod = dout("o_mod", [128, 32]); o_uT = dout("o_uT", [128, 8 * 2048], BF16); o_gtm = dout("o_gtm", [128, 1024])
        DMA("sp", [D_mod], [], "dbg", dma(o_mod, modT[:]))
        DMA("sp", [D_gtm], [], "dbg", dma(o_gtm, gtm_bc[:]))
        DMA("sp", D_uT, [], "dbg", dma(o_uT, uT[:].rearrange("p a b -> p (a b)")))
        return finish(nc, P, ["dbg"]), dbg
    def issue_kv_gathers(first):
        if first:
            DMA("pool", [D_Lfd], [D_Lfall], "cc_lf", cc(Lf_loc, Lf_all), inc=1, nofence=True)
        for g in ([0] if first else [1, 2, 3]):
            DMA("pool", [D_Kd], [D_Kall[g]], "cc_k", cc(Kd_loc[g], Kd_all[g]), inc=1, nofence=True)
            DMA("pool", [D_Vd], [D_Vall[g]], "cc_v", cc(Vd_loc[g], Vd_all[g]), inc=1, nofence=True)
    issue_kv_gathers(True)
    if stage == 1:
        issue_kv_gathers(False)

    if stage == 1:
        o_q = dout("o_q", [512, 2048], BF16)
        o_k = dout("o_kall", [512, 2048], BF16)
        o_v = dout("o_vall", [8192, 130], BF16)
        o_lf = dout("o_lfall", [32, 2048])
        o_xbh = dout("o_xbh", [128, 4 * 16 * 131])
        o_ggb = dout("o_ggb", [128, 4 * 2048], BF16)
        DMA("sp", [D_Qd], [], "dbg", dma(o_q, Qd))
        DMA("sp", [D_Kall[1]], [], "dbg", dma(o_k, Kd_all[1]))
        DMA("sp", [D_Vall[1]], [], "dbg", dma(o_v, Vd_all[1]))
        DMA("sp", [D_Lfall], [], "dbg", dma(o_lf, Lf_all))
        DMA("sp", [D_xbh], [], "dbg", dma(o_xbh, xbh[:].rearrange("p a b c -> p (a b c)")))
        DMA("sp", [D_ggb], [], "dbg", dma(o_ggb, ggb[:].rearrange("p a b -> p (a b)")))
        return finish(nc, P, ["dbg"]), dbg

    P.fence()
    A.top = m1b
    lruT = A.take([128, 4, 2048], BF16)
    D_lruT = Dep("lruT")
    mL = A.top
    cw = A.take([128, 4, 4], F32)
    cbv = A.take([128, 4], F32)
    brg = A.take([128, 4], F32)
    big = A.take([128, 4], F32)
    lam = A.take([128, 4], F32)
    cneg = A.take([128, 4], F32)
    cneg2 = A.take([128, 4], F32)
    wbd_f = A.take([128, 2, 4, 128], F32)
    wbd = A.take([128, 2, 4, 128], BF16)
    xc = A.take([128, 2048], F32)
    xcb = A.take([128, 2048], BF16)
    rr = A.take([128, 2048], F32)
    igt = A.take([128, 2048], F32)
    at2 = [A.take([128, 2048], F32) for _ in range(2)]
    t1 = A.take([128, 2048], F32)
    ut2 = [A.take([128, 2048], F32) for _ in range(2)]
    ht = A.take([128, 2048], F32)
    bl2 = [A.take([128, 32], F32) for _ in range(2)]
    ball2 = [A.take([128, 4, 32], F32) for _ in range(2)]
    D_at2 = [Dep("at0"), Dep("at1")]
    D_ut2 = [Dep("ut0"), Dep("ut1")]
    D_bl2 = [Dep("bl0"), Dep("bl1")]
    D_ball2 = [Dep("ball0"), Dep("ball1")]
    Ag = A.take([128, 64], F32)
    Hg = A.take([128, 64], F32)
    Sx = A.take([128, 68], F32)
    hin = A.take([128, 16], F32)
    tmp64 = A.take([128, 16, 4], F32)
    D_lc = Dep("lruconst")
    DMA("sp", [], [D_lc], "c_l",
        *[dma_nc(cw[:, :, w], conv_w[w].rearrange("(c p) -> p c", p=128)) for w in range(4)],
        dma_nc(cbv[:], conv_b.rearrange("(c p) -> p c", p=128)),
        dma_nc(brg[:], b_rg.rearrange("(c p) -> p c", p=128)),
        dma_nc(big[:], b_ig.rearrange("(c p) -> p c", p=128)),
        dma_nc(lam[:], lru_lambda.rearrange("(c p) -> p c", p=128)))
    OP("pool", [], [D_lc], lambda e: e.memset(wbd_f[:], 0.0))
    fl = []
    for gi, wsrc in enumerate((w_rg, w_ig)):
        for cch in range(4):
            for hh in range(2):
                fl.append(dma(wbd_f[hh * 64:(hh + 1) * 64, gi, cch, hh * 64:(hh + 1) * 64], wsrc[cch * 2 + hh]))
    DMA("sp", [D_lc], [D_lc], "c_l2", *fl)
    OP("dve", [D_lc], [D_lc], lambda e: e.tensor_copy(out=wbd[:], in_=wbd_f[:]))
    OP("act", [D_lc], [D_lc], lambda e: e.activation(out=cneg[:], in_=lam[:], func=AF.Exp, scale=-1.0))
    OP("act", [D_lc], [D_lc], lambda e: e.activation(out=cneg[:], in_=cneg[:], func=AF.Ln, bias=1.0, scale=1.0))
    CHAIN("dve", [D_lc], [D_lc],
          lambda e: e.tensor_scalar(out=cneg2[:], in0=cneg[:], scalar1=-16.0, scalar2=None, op0=ALU.mult),
          lambda e: e.tensor_scalar(out=cneg[:], in0=cneg[:], scalar1=-8.0, scalar2=None, op0=ALU.mult))
    D_l = {k: Dep(k) for k in "xc xcb rr ig at t1 ut ht bl ball ag sx hin pg0 pg1".split()}
    Bd_l = [nc.dram_tensor(f"Bd_l{c}", [128, 32], F32).ap() for c in range(4)]
    Bd_a = [nc.dram_tensor(f"Bd_a{c}", [512, 32], F32).ap() for c in range(4)]
    D_Bd = [Dep(f"Bd{c}") for c in range(4)]
    D_Ba = [Dep(f"Ba{c}") for c in range(4)]

    def lru_pass1(c):
        at, ut, bl = at2[c % 2], ut2[c % 2], bl2[c % 2]
        D_at, D_ut, D_bl = D_at2[c % 2], D_ut2[c % 2], D_bl2[c % 2]
        xv = xbh[:, c]
        xc3 = xc[:].rearrange("p (m w) -> p m w", w=128)
        CHAIN("dve", [D_xbh, D_lc], [D_l["xc"]],
           lambda e: e.tensor_scalar(out=xc3, in0=xv[:, :, 0:128], scalar1=cw[:, c, 0:1], scalar2=cbv[:, c:c + 1], op0=ALU.mult, op1=ALU.add),
           *[(lambda w=w: lambda e: e.scalar_tensor_tensor(out=xc3, in0=xv[:, :, w:w + 128], scalar=cw[:, c, w:w + 1], in1=xc3,
                                                           op0=ALU.mult, op1=ALU.add))() for w in (1, 2, 3)])
        OP("act", [D_l["xc"]], [D_l["xcb"]], lambda e: e.activation(out=xcb[:], in_=xc[:], func=AF.Copy))
        for T in range(4):
            tc_ = slice(T * 512, (T + 1) * 512)
            for gi, (dst, bia, nm) in enumerate(((rr, brg, "rr"), (igt, big, "ig"))):
                pb = banks[gi * 2 + T % 2]
                D_p = D_l["pg0"] if gi == 0 else D_l["pg1"]
                OP("pe", [D_l["xcb"], D_lc], [D_p], mm(pb[:, :], wbd[:, gi, c, :], xcb[:, tc_], True, True))
                OP("act", [D_p, D_lc], [], (lambda pb=pb, dst=dst, bia=bia, tc_=tc_: lambda e: e.activation(
                    out=dst[:, tc_], in_=pb[:, :], func=AF.Sigmoid, bias=bia[:, c:c + 1], scale=1.0))(), Wp=[D_l[nm]])
        OP("act", [D_l["rr"], D_lc], [D_at], lambda e: e.activation(out=at[:], in_=rr[:], func=AF.Exp, scale=cneg[:, c:c + 1]))
        OP("act", [D_l["rr"], D_lc], [D_l["t1"]], lambda e: e.activation(out=t1[:], in_=rr[:], func=AF.Exp, scale=cneg2[:, c:c + 1]))
        OP("act", [D_l["t1"]], [D_l["t1"]], lambda e: e.activation(out=t1[:], in_=t1[:], func=AF.Sqrt, bias=1.0, scale=-1.0))
        CHAIN("dve", [D_l["t1"], D_l["ig"], D_l["xc"]], [D_ut],
           lambda e: e.tensor_tensor(out=ut[:], in0=t1[:], in1=igt[:], op=ALU.mult),
           lambda e: e.tensor_tensor(out=ut[:], in0=ut[:], in1=xc[:], op=ALU.mult))
        OP("dve", [D_at, D_ut], [D_l["ht"]],
           *[(lambda m=m: lambda e: e.tensor_tensor_scan(out=ht[:, m * 128:(m + 1) * 128], data0=at[:, m * 128:(m + 1) * 128],
                                                         data1=ut[:, m * 128:(m + 1) * 128], initial=0.0, op0=ALU.mult, op1=ALU.add))()
             for m in range(16)])
        OP("dve", [D_l["ht"], D_l["rr"]], [D_bl],
           lambda e: e.tensor_copy(out=bl[:, 16:32], in_=ht[:].rearrange("p (m w) -> p m w", w=128)[:, :, 127]),
           lambda e: e.reduce_sum(out=bl[:, 0:16], in_=rr[:].rearrange("p (m w) -> p m w", w=128), axis=AX.X))
        OP("act", [D_bl, D_lc], [D_bl], lambda e: e.activation(out=bl[:, 0:16], in_=bl[:, 0:16], func=AF.Exp, scale=cneg[:, c:c + 1]))
        DMA("sp", [D_bl], [D_Bd[c]], "st_b", dma(Bd_l[c], bl[:]))
        DMA("pool", [D_Bd[c]], [D_Ba[c]], "cc_b", cc(Bd_l[c], Bd_a[c]), inc=1)

    def lru_pass2(c):
        at, ut, ball = at2[c % 2], ut2[c % 2], ball2[c % 2]
        D_at, D_ut, D_ball = D_at2[c % 2], D_ut2[c % 2], D_ball2[c % 2]
        DMA("sp", [D_Ba[c]], [D_ball], "ld_b", dma(ball[:], Bd_a[c].rearrange("(r p) f -> p r f", p=128)))
        OP("dve", [D_ball], [D_l["ag"]],
           lambda e: e.tensor_copy(out=Ag[:].rearrange("p (m r) -> p r m", r=4), in_=ball[:, :, 0:16]),
           lambda e: e.tensor_copy(out=Hg[:].rearrange("p (m r) -> p r m", r=4), in_=ball[:, :, 16:32]))
        OP("dve", [D_l["ag"]], [D_l["sx"]],
           lambda e: e.memset(Sx[:, 0:1], 0.0),
           lambda e: e.tensor_tensor_scan(out=Sx[:, 1:65], data0=Ag[:], data1=Hg[:], initial=0.0, op0=ALU.mult, op1=ALU.add))
        CHAIN("dve", [D_l["sx"], D_oh], [D_l["hin"]],
           lambda e: e.tensor_tensor(out=tmp64[:], in0=Sx[:, 0:64].rearrange("p (m r) -> p m r", r=4),
                                     in1=oh[:].unsqueeze(1).to_broadcast([128, 16, 4]), op=ALU.mult),
           lambda e: e.reduce_sum(out=hin[:], in_=tmp64[:], axis=AX.X))
        OP("dve", [D_at, D_ut, D_l["hin"]], [D_l["ht"]],
           *[(lambda m=m: lambda e: e.tensor_tensor_scan(out=ht[:, m * 128:(m + 1) * 128], data0=at[:, m * 128:(m + 1) * 128],
                                                         data1=ut[:, m * 128:(m + 1) * 128], initial=hin[:, m:m + 1], op0=ALU.mult, op1=ALU.add))()
             for m in range(16)])
        OP("dve", [D_l["ht"], D_ggb], [], lambda e: e.tensor_tensor(out=lruT[:, c, :], in0=ht[:], in1=ggb[:, c, :], op=ALU.mult), Wp=[D_lruT])

    lru_pass1(0)
    for c in range(4):
        if c + 1 < 4:
            lru_pass1(c + 1)
        lru_pass2(c)
    issue_kv_gathers(False)

    if stage == 2:
        o_l = dout("o_lru", [128, 4 * 2048], BF16)
        DMA("sp", [D_lruT], [], "dbg", dma(o_l, lruT[:].rearrange("p a b -> p (a b)")))
        return finish(nc, P, ["dbg"]), dbg

    P.fence()
    A.top = mL
    OnT = A.take([64, 8, 2048], BF16)
    D_OnT = Dep("OnT")
    mC = A.top
    Kh = [A.take([67, 8192], BF16) for _ in range(2)]
    Vh = [A.take([128, 64, 65], BF16) for _ in range(2)]
    Qh = [A.take([67, 2048], BF16) for _ in range(2)]
    pTt = [A.take([128, 512], BF16) for _ in range(4)]
    Osb = [A.take([65, 512], F32) for _ in range(2)]
    rs = [A.take([65, 512], F32) for _ in range(2)]
    D_Kh = [Dep("Kh0"), Dep("Kh1")]
    D_Vh = [Dep("Vh0"), Dep("Vh1")]
    D_Qh = [Dep("Qh0"), Dep("Qh1")]
    for s_ in range(2):
        OP("pool", [], [D_Kh[s_]], (lambda s_=s_: lambda e: e.memset(Kh[s_][64:67, :], 1.0))())

    def attn_load_kv(h, q="sp"):
        s_ = h % 2
        g = h // 2
        hh = h % 2
        Kv = Kh[s_][0:64, :].rearrange("d (m r p) -> d m r p", r=4, p=128)
        Vv = Vh[s_][:].rearrange("p (m r) d -> p m r d", r=4)
        DMA(q, [D_Kall[g]], [], f"ld_k{s_}",
            *[dma(Kv[:, :, r, :], Kd_all[g][r * 128 + hh * 64:r * 128 + hh * 64 + 64, :].rearrange("d (m p) -> d m p", p=128))
              for r in range(4)], Wp=[D_Kh[s_]])
        DMA(q, [D_Vall[g]], [D_Vh[s_]], f"ld_v{s_}",
            *[dma(Vv[:, :, r, :], Vd_all[g][r * 2048:(r + 1) * 2048, hh * 65:(hh + 1) * 65].rearrange("(m p) d -> p m d", p=128))
              for r in range(4)])
    attn_load_kv(0, "act")
    attn_load_kv(1, "act")
    mAttnEnd = A.top
    ATOP = Arena(arena_t, ARENA_SZ)
    ATOP.top = ARENA_SZ - 21 * 1024
    assert mAttnEnd <= ATOP.top, (mAttnEnd, ATOP.top)
    A2 = Arena(arena_t, m1b)
    A2.top = m1
    lfg = A2.take([8, 8192], F32)
    cnq = A2.take([8, 2048], F32)
    cnT = A2.take([128, 64, 8], F32)
    oh8 = A2.take([8, 4], F32)
    maskT = A2.take([128, 4, 128], BF16)
    r1 = ATOP.take([8, 2048], F32)
    aug = ATOP.take([8, 3, 2048], BF16)
    D_lfg = Dep("lfg")
    D_cnq = Dep("cnq")
    D_aug = Dep("aug")
    D_cnT = Dep("cnT")
    D_mask = Dep("mask")
    D_pc = Dep("pc")
    D_Cq = Dep("Cq")
    lfg4 = lfg[:].rearrange("h (m r p) -> h m r p", r=4, p=128)
    DMA("sp", [D_Lfall], [D_lfg], "ld_lf",
        *[dma(lfg4[:, :, r, :], Lf_all[r * 8:(r + 1) * 8, :].rearrange("h (m p) -> h m p", p=128)) for r in range(4)])
    DMA("sp", [], [D_mask], "c_mask", dma(maskT[:], mask_in), dma(oh8[:], oh_in[0:8, :]))
    OP("dve", [D_lfg, D_cnq], [D_lfg],
       lambda e: e.tensor_tensor_scan(out=lfg[:], data0=ones_f[0:8, 0:1].to_broadcast([8, 8192]), data1=lfg[:], initial=0.0,
                                      op0=ALU.mult, op1=ALU.add))
    cnq3 = cnq[:].rearrange("h (m p) -> h m p", p=128)
    CHAIN("dve", [D_lfg, D_mask], [D_cnq],
          lambda e: e.tensor_scalar(out=cnq3, in0=lfg4[:, :, 0, :], scalar1=oh8[:, 0:1], scalar2=None, op0=ALU.mult),
          *[(lambda r=r: lambda e: e.scalar_tensor_tensor(out=cnq3, in0=lfg4[:, :, r, :], scalar=oh8[:, r:r + 1], in1=cnq3,
                                                          op0=ALU.mult, op1=ALU.add))() for r in (1, 2, 3)])
    CHAIN("dve", [D_cnq], [D_aug],
          lambda e: e.tensor_scalar(out=aug[:, 0, :], in0=cnq[:], scalar1=-1.0, scalar2=None, op0=ALU.mult),
          lambda e: e.scalar_tensor_tensor(out=r1[:], in0=cnq[:], scalar=-1.0, in1=aug[:, 0, :], op0=ALU.mult, op1=ALU.subtract),
          lambda e: e.tensor_copy(out=aug[:, 1, :], in_=r1[:]),
          lambda e: e.tensor_tensor(out=r1[:], in0=r1[:], in1=aug[:, 1, :], op=ALU.subtract),
          lambda e: e.tensor_copy(out=aug[:, 2, :], in_=r1[:]))
    DMA("sp", [D_aug], [D_Cq], "st_cq", dma(Cq, aug[:]))
    pc = banks[7]
    OP("pe", [D_lfg, D_const], [D_pc],
       *[(lambda i=i: lambda e: e.transpose(out=pc[:, i * 8:(i + 1) * 8], in_=lfg[:, i * 128:(i + 1) * 128], identity=ident_f[0:8, 0:8]))()
         for i in range(64)])
    OP("act", [D_pc], [D_cnT], lambda e: e.activation(out=cnT[:].rearrange("p i h -> p (i h)"), in_=pc[:, :], func=AF.Copy))

    if stage == 25:
        o_cn = dout("o_cn", [8, 8192]); o_cnT = dout("o_cnT", [128, 512]); o_aug = dout("o_aug", [8, 3 * 2048], BF16)
        DMA("sp", [D_lfg], [], "dbg", dma(o_cn, lfg[:]))
        DMA("sp", [D_cnT], [], "dbg", dma(o_cnT, cnT[:].rearrange("p i h -> p (i h)")))
        DMA("sp", [D_aug], [], "dbg", dma(o_aug, aug[:].rearrange("h a t -> h (a t)")))
        return finish(nc, P, ["dbg"]), dbg

    D_pS = [Dep(f"pS{i}") for i in range(4)]
    D_pTt = [Dep(f"pTt{i}") for i in range(4)]
    D_pO = [Dep("pO0"), Dep("pO1")]
    D_Osb = [Dep("Osb0"), Dep("Osb1")]
    D_rs = [Dep("rs0"), Dep("rs1")]
    D_pB = Dep("pB")
    slot_ctr = [0]

    def attn_head(h):
        s_ = h % 2
        if h >= 2:
            attn_load_kv(h)
        DMA("sp", [D_Qd, D_Cq], [D_Qh[s_]], f"ld_q{s_}",
            dma(Qh[s_][0:64, :], Qd[h * 64:(h + 1) * 64, :]), dma(Qh[s_][64:67, :], Cq[h]))
        def do_qt(qt):
            items = [(kb, 0, None) for kb in range(16 * qt)]
            for dd in range(4):
                for r in range(4):
                    items.append(((4 * qt + dd) * 4 + r, 128 * dd, r))
            o = (h * 4 + qt) % 2
            pO = banks[4 + o]
            n = len(items)
            slots = {}

            def emit_S(idx):
                kb, c0, r = items[idx]
                sl = slot_ctr[0] % 4
                slot_ctr[0] += 1
                slots[idx] = sl
                pS = banks[sl]
                fns = [mm(pS[:, c0:512], Kh[s_][:, kb * 128:(kb + 1) * 128], Qh[s_][:, qt * 512 + c0:(qt + 1) * 512], True, True)]
                R = [D_Kh[s_], D_Qh[s_]]
                if r is not None:
                    fns.append(lambda e: e.matmul(pS[:, c0:c0 + 128], lhsT=ident_bf[:], rhs=maskT[:, r, :], start=False, stop=True,
                                                  skip_group_check=True))
                    R = R + [D_mask, D_const]
                OP("pe", R, [D_pS[sl]], *fns)

            def emit_rest(idx):
                kb, c0, r = items[idx]
                sl = slots[idx]
                pS = banks[sl]
                OP("act", [D_pS[sl], D_cnT], [D_pTt[sl]], lambda e: e.activation(
                    out=pTt[sl][:, c0:512], in_=pS[:, c0:512], func=AF.Exp, bias=cnT[:, kb, h:h + 1], scale=1.0))
                OP("pe", [D_pTt[sl], D_Vh[s_]], [], mm(pO[0:65, c0:512], Vh[s_][:, kb, :], pTt[sl][:, c0:512], idx == 0, idx == n - 1),
                   Wp=[D_pO[o]])

            emit_S(0)
            if n > 1:
                emit_S(1)
            for idx in range(n):
                if idx + 2 < n:
                    emit_S(idx + 2)
                emit_rest(idx)
            OP("act", [D_pO[o]], [D_Osb[o]], lambda e: e.activation(out=Osb[o][:, :], in_=pO[0:65, :], func=AF.Copy))
            OP("dve", [D_Osb[o]], [D_rs[o]], lambda e: e.reciprocal(out=rs[o][64:65, :], in_=Osb[o][64:65, :]))
            pB = banks[6]
            OP("pe", [D_rs[o], D_const], [D_pB], mm(pB[0:64, :], ones_f[64:65, 0:64], rs[o][64:65, :], True, True))
            OP("dve", [D_Osb[o], D_pB], [], lambda e: e.tensor_tensor(
                out=OnT[:, h, qt * 512:(qt + 1) * 512], in0=Osb[o][0:64, :], in1=pB[0:64, :], op=ALU.mult), Wp=[D_OnT])

        for qt in range(4):
            do_qt(qt)

    for m in range(3):
        for jj in range(4):
            DMA("pool", [D_Wbl[m]], [], "cc_w", cc(Wb_loc[m][jj * 128:(jj + 1) * 128, :], Wb_all[m][jj * 512:(jj + 1) * 512, :]),
                inc=1, nofence=True, Wp=[D_Wall[m]])
    for h in range(8):
        attn_head(h)

    if stage == 3:
        o_on = dout("o_on", [64, 8 * 2048], BF16)
        DMA("sp", [D_OnT], [], "dbg", dma(o_on, OnT[:].rearrange("p a b -> p (a b)")))
        return finish(nc, P, ["dbg"]), dbg

    P.fence()
    X1d = nc.dram_tensor("X1d", [2048, 1024], F32).ap()
    A2 = Arena(arena_t, m1b)
    A2.top = m1
    WoA = A2.take([64, 8, 1024], BF16)
    WoL = A2.take([128, 4, 1024], BF16)
    NW4 = 4
    xt4 = [A2.take([128, 1024], F32) for _ in range(NW4)]
    yv = [A2.take([128, 1024], F32) for _ in range(NW4)]
    A.top = mC
    xn2_all = A.take([128, 16, 1024], BF16)
    NB = 16
    RT = {k: A.take([128, NB, n], F32) for k, n in
          (("lg", 20), ("gmax", 1), ("ge", 4), ("gsum", 1), ("grpw", 1), ("gsel", 4), ("tmp", 16), ("sel", 4), ("m1", 1),
           ("mask1", 4), ("sel2", 4), ("m2", 1), ("mask2", 4), ("d21", 1), ("e2", 1), ("den", 1), ("w1", 1), ("w2", 1))}
    posA_i = A.take([128, 16], I32)
    posB_i = A.take([128, 16], I32)
    eidx_i = A.take([128, 32], I32)
    mKeep = A.top
    u2T = A.take([128, 8, 2048], BF16)
    mSort = A.top - 32 * 1024
    junk = [A.take([128, 2, 128], F32) for _ in range(NW4)]
    g1_bc = A.take([128, 1024], F32)
    b1_bc = A.take([128, 1024], F32)
    gaT = A.take([64, 8], F32)
    glT = A.take([128, 4], F32)
    wr = A.take([128, 8, 20], BF16)
    br_bc = A.take([128, 20], F32)
    lnsc4 = [mk_scratch_in(A) for _ in range(NW4)]
    ss = [A.take([128, 2], F32) for _ in range(NW4)]
    sd2 = [A.take([128, 2], F32) for _ in range(NW4)]
    rr2 = [A.take([128, 2], F32) for _ in range(NW4)]
    nmr4 = [A.take([128, 1], F32) for _ in range(NW4)]
    D_u2T = [Dep(f"u2T{b}") for b in range(16)]
    D_Wo = Dep("Wo")
    D_bc1 = Dep("bc1")
    D_wr = Dep("wr")
    D_xt4 = [Dep(f"xt4{i}") for i in range(NW4)]
    D_yv = [Dep(f"yv{i}") for i in range(NW4)]
    D_xn2b = [Dep(f"xn2b{b}") for b in range(16)]
    D_pG = Dep("pG")
    D_pYA = [Dep("pYA0"), Dep("pYA1")]
    D_pYL = [Dep("pYL0"), Dep("pYL1")]
    D_pT4 = Dep("pT4")
    D_pR = Dep("pR")
    D_ss = [Dep(f"ss{i}") for i in range(NW4)]
    D_rt = [Dep("rt0"), Dep("rt1")]
    D_X1d = Dep("X1d")
    DMA("pool", [], [D_Wo], "ld_wo",
        dma(WoA[:], w_out[0:512, :].rearrange("(h d) e -> d h e", d=64)),
        dma(WoL[:], w_out[512:1024, :].rearrange("(c p) e -> p c e", p=128)))
    DMA("sp", [], [D_Wo], "c_g",
        dma_nc(gaT[:], g_attn.rearrange("(h d) -> d h", d=64)), dma_nc(glT[:], g_lru.rearrange("(c p) -> p c", p=128)))
    DMA("sp", [], [D_bc1], "c_bc1", dma(g1_bc[:], ln1_g.partition_broadcast(128)), dma(b1_bc[:], ln1_b.partition_broadcast(128)),
        dma(br_bc[:, 0:4], b_grp.partition_broadcast(128)), dma(br_bc[:, 4:20], b_exp.partition_broadcast(128)))
    DMA("pool", [], [D_wr], "ld_wr",
        dma_nc(wr[:, :, 0:4], w_grp.rearrange("(k p) g -> p k g", p=128)), dma_nc(wr[:, :, 4:20], w_exp.rearrange("(k p) g -> p k g", p=128)))
    for h in range(8):
        OP("dve", [D_Wo, D_gtm], [D_Wo], (lambda h=h: lambda e: e.scalar_tensor_tensor(
            out=WoA[:, h, :], in0=WoA[:, h, :], scalar=gaT[:, h:h + 1], in1=gtm_bc[0:64, :], op0=ALU.mult, op1=ALU.mult))())
    for c in range(4):
        OP("dve", [D_Wo, D_gtm], [D_Wo], (lambda c=c: lambda e: e.scalar_tensor_tensor(
            out=WoL[:, c, :], in0=WoL[:, c, :], scalar=glT[:, c:c + 1], in1=gtm_bc[:], op0=ALU.mult, op1=ALU.mult))())
    D_X4 = [Dep(f"bX{i}") for i in range(NW4)]
    D_Y4 = [Dep(f"bY{i}") for i in range(NW4)]

    def post_block(blk):
        s = blk % NW4
        bc_ = slice(blk * 128, (blk + 1) * 128)
        DMA("sp", [], [D_xt4[s]], f"xt4{s}", dma(xt4[s][:], x_loc[blk * 128:(blk + 1) * 128, :]))
        pG = banks[s]
        OP("pe", [D_OnT, D_lruT], [D_X4[s]],
           *[mm(pG[:, 0:128], OnT[:, h, bc_], OnT[:, h, bc_], h == 0, h == 7) for h in range(8)],
           *[mm(pG[:, 128:256], lruT[:, c, bc_], lruT[:, c, bc_], c == 0, c == 3) for c in range(4)])
        yield
        OP("dve", [D_X4[s], D_const], [D_ss[s]], lambda e: e.tensor_tensor(
            out=junk[s][:], in0=pG[:, 0:256].rearrange("p (a b) -> p a b", a=2),
            in1=ident_f[:].unsqueeze(1).to_broadcast([128, 2, 128]), op=ALU.mult))
        yield
        OP("dve", [D_ss[s]], [D_ss[s]], lambda e: e.reduce_sum(out=ss[s][:], in_=junk[s][:], axis=AX.X))
        yield
        OP("act", [D_ss[s]], [D_ss[s]], lambda e: e.activation(out=sd2[s][:], in_=ss[s][:], func=AF.Sqrt, bias=1e-6, scale=1.0 / 512))
        yield
        OP("dve", [D_ss[s]], [D_ss[s]], lambda e: e.reciprocal(out=rr2[s][:], in_=sd2[s][:]))
        yield
        for half in range(2):
            hc = slice(half * 512, (half + 1) * 512)
            pYA = banks[4 + s]
            pYL = banks[4 + s]
            OP("pe", [D_OnT, D_Wo], [D_Y4[s]], *[mm(pYA[:, :], OnT[:, h, bc_], WoA[:, h, hc], h == 0, h == 7) for h in range(8)])
            yield
            OP("act", [D_Y4[s], D_ss[s]], [], (lambda pYA=pYA, hc=hc: lambda e: e.activation(
                out=yv[s][:, hc], in_=pYA[:, :], func=AF.Copy, scale=rr2[s][:, 0:1]))(), Wp=[D_yv[s]])
            yield
            OP("pe", [D_lruT, D_Wo], [D_Y4[s]], *[mm(pYL[:, :], lruT[:, c, bc_], WoL[:, c, hc], c == 0, c == 3) for c in range(4)])
            yield
            OP("dve", [D_Y4[s], D_ss[s], D_yv[s]], [], (lambda pYL=pYL, hc=hc: lambda e: e.scalar_tensor_tensor(
                out=yv[s][:, hc], in0=pYL[:, :], scalar=rr2[s][:, 1:2], in1=yv[s][:, hc], op0=ALU.mult, op1=ALU.add))(), Wp=[D_yv[s]])
            yield
        OP("dve", [D_yv[s], D_xt4[s]], [D_yv[s]], lambda e: e.scalar_tensor_tensor(
            out=yv[s][:], in0=xt4[s][:], scalar=ALPHA, in1=yv[s][:], op0=ALU.mult, op1=ALU.add))
        yield
        for _ in ln_apply_gen(yv[s][:], 128, D_yv[s], yv[s][:], D_yv[s], lnsc4[s], nmr=nmr4[s]):
            yield
        OP("pool", [D_yv[s], D_bc1], [D_yv[s]], lambda e: e.tensor_tensor(out=yv[s][:], in0=yv[s][:], in1=g1_bc[:], op=ALU.mult))
        yield
        OP("pool", [D_yv[s], D_bc1], [D_yv[s]], lambda e: e.tensor_tensor(out=yv[s][:], in0=yv[s][:], in1=b1_bc[:], op=ALU.add))
        yield
        for _ in ln_apply_gen(yv[s][:], 128, D_yv[s], xn2_all[:, blk, :], D_xn2b[blk], lnsc4[s], nmr=nmr4[s]):
            yield
        OP("act", [D_yv[s], D_xt4[s]], [D_xt4[s]], lambda e: e.activation(out=xt4[s][:], in_=yv[s][:], func=AF.Copy, scale=ALPHA))
        DMA("sp", [D_xt4[s]], [], "st_x1", dma(X1d[blk * 128:(blk + 1) * 128, :], xt4[s][:]), Wp=[D_X1d])
        pT = bank_bf(s)
        OP("pe", [D_xn2b[blk], D_const], [D_X4[s]],
           *[(lambda k=k: lambda e: e.transpose(out=pT[:, k, :], in_=xn2_all[:, blk, k * 128:(k + 1) * 128], identity=ident_bf[:]))() for k in range(8)])
        yield
        OP("act", [D_X4[s], D_mod], [], *[(lambda k=k: lambda e: e.activation(
            out=u2T[:, k, bc_], in_=pT[:, k, :], func=AF.Identity, scale=sc_f1[:, k:k + 1], bias=sh_f[:, k:k + 1]))() for k in range(0, 8, 2)],
            Wp=[D_u2T[blk]])
        OP("dve", [D_X4[s], D_mod], [], *[(lambda k=k: lambda e: e.tensor_scalar(
            out=u2T[:, k, bc_], in0=pT[:, k, :], scalar1=sc_f1[:, k:k + 1], scalar2=sh_f[:, k:k + 1], op0=ALU.mult, op1=ALU.add))()
            for k in range(1, 8, 2)], Wp=[D_u2T[blk]])
        yield

    def interleave(gens):
        gens = list(gens)
        while gens:
            for g_ in list(gens):
                try:
                    next(g_)
                except StopIteration:
                    gens.remove(g_)

    Xs = nc.dram_tensor("Xs", [NSLOT, 1024], BF16).ap()
    D_Xs = Dep("Xs")
    NZ = NSLOT // 128
    Xs_v = Xs.rearrange("(p a) d -> p a d", p=128)
    lru_rows = lruT[:].rearrange("p c (a d) -> p (c a) d", d=1024)
    DMA("sp", [D_lruT], [D_Xs], "z_xs", *[dma(Xs_v[:, a0:min(a0 + 8, NZ), :], lru_rows[:, 0:min(8, NZ - a0), :]) for a0 in range(0, NZ, 8)])
    for b0 in range(0, 16, NW4):
        interleave([post_block(b0 + i) for i in range(NW4)])

    pR = banks[0]
    D_pR = Dep("pR")
    OP("pe", D_u2T + [D_wr], [D_pR],
       *[mm(pR[:, blk * 20:(blk + 1) * 20], u2T[:, k, blk * 128:(blk + 1) * 128], wr[:, k, :], k == 0, k == 7)
         for blk in range(16) for k in range(8)])
    D_r = Dep("router")

    def bcl(ap, n):
        return ap.to_broadcast([128, NB, n])
    lg = RT["lg"]
    lgg = lg[:, :, 0:4]
    steps = [
        ("dve", lambda e: e.tensor_tensor(out=lg[:], in0=pR[:, 0:NB * 20].rearrange("p (b n) -> p b n", n=20),
                                          in1=br_bc[:].unsqueeze(1).to_broadcast([128, NB, 20]), op=ALU.add)),
        ("dve", lambda e: e.reduce_max(out=RT["gmax"][:].rearrange("p b o -> p (b o)"), in_=lgg, axis=AX.X)),
        ("dve", lambda e: e.tensor_tensor(out=RT["ge"][:], in0=lgg, in1=bcl(RT["gmax"][:], 4), op=ALU.subtract)),
        ("act", lambda e: e.activation(out=RT["ge"][:], in_=RT["ge"][:], func=AF.Exp)),
        ("dve", lambda e: e.reduce_sum(out=RT["gsum"][:].rearrange("p b o -> p (b o)"), in_=RT["ge"][:], axis=AX.X)),
        ("dve", lambda e: e.reciprocal(out=RT["grpw"][:], in_=RT["gsum"][:])),
        ("dve", lambda e: e.tensor_tensor(out=RT["gsel"][:], in0=lgg, in1=bcl(RT["gmax"][:], 4), op=ALU.is_equal)),
    ]
    tmpv = RT["tmp"][:].rearrange("p b (e g) -> p b e g", g=4)
    elv = lg[:, :, 4:20].rearrange("p b (g e) -> p b e g", e=4)
    for ee in range(4):
        steps.append(("dve", (lambda ee=ee: lambda e: e.tensor_tensor(out=tmpv[:, :, ee, :], in0=elv[:, :, ee, :], in1=RT["gsel"][:], op=ALU.mult))()))
    steps += [
        ("dve", lambda e: e.reduce_sum(out=RT["sel"][:].rearrange("p b e -> p (b e)"),
                                       in_=RT["tmp"][:].rearrange("p b (e g) -> p (b e) g", g=4), axis=AX.X)),
        ("dve", lambda e: e.reduce_max(out=RT["m1"][:].rearrange("p b o -> p (b o)"), in_=RT["sel"][:], axis=AX.X)),
        ("dve", lambda e: e.tensor_tensor(out=RT["mask1"][:], in0=RT["sel"][:], in1=bcl(RT["m1"][:], 4), op=ALU.is_equal)),
        ("dve", lambda e: e.scalar_tensor_tensor(out=RT["sel2"][:], in0=RT["mask1"][:], scalar=-1e30, in1=RT["sel"][:], op0=ALU.mult, op1=ALU.add)),
        ("dve", lambda e: e.reduce_max(out=RT["m2"][:].rearrange("p b o -> p (b o)"), in_=RT["sel2"][:], axis=AX.X)),
        ("dve", lambda e: e.tensor_tensor(out=RT["mask2"][:], in0=RT["sel2"][:], in1=bcl(RT["m2"][:], 4), op=ALU.is_equal)),
        ("dve", lambda e: e.tensor_tensor(out=RT["d21"][:], in0=RT["m2"][:], in1=RT["m1"][:], op=ALU.subtract)),
        ("act", lambda e: e.activation(out=RT["e2"][:], in_=RT["d21"][:], func=AF.Exp)),
        ("dve", lambda e: e.tensor_scalar(out=RT["den"][:], in0=RT["e2"][:], scalar1=1.0, scalar2=None, op0=ALU.add)),
        ("dve", lambda e: e.reciprocal(out=RT["den"][:], in_=RT["den"][:])),
        ("dve", lambda e: e.tensor_tensor(out=RT["w1"][:], in0=RT["den"][:], in1=RT["grpw"][:], op=ALU.mult)),
        ("dve", lambda e: e.tensor_tensor(out=RT["w2"][:], in0=RT["w1"][:], in1=RT["e2"][:], op=ALU.mult)),
    ]
    for i, (eng, f) in enumerate(steps):
        P.op(eng, (lambda f=f: lambda e: [f(e)])(), R=[D_r] + ([D_pR, D_bc1] if i == 0 else []), W=[D_r])

    P.fence()
    AS = Arena(arena_t, mSort + 32 * 1024)
    AS.top = mSort
    cstb = AS.take([128, 256], BF16)
    cstf = AS.take([128, 80], F32)
    oh1 = AS.take([128, NB, 16], F32)
    oh2 = AS.take([128, NB, 16], F32)
    Mb = AS.take([128, 256], BF16)
    wt = AS.take([128, 2, NB, 16], F32)
    exb = AS.take([128, NB, 16], F32)
    posf = AS.take([128, NB, 16], F32)
    ptmp = AS.take([128, NB, 16], F32)
    cmpA = AS.take([128, 16, 16], F32)
    cmpB = AS.take([128, 32, 16], F32)
    cnt = AS.take([128, 16], F32)
    ntl = AS.take([128, 16], F32)
    incl = AS.take([128, 16], F32)
    basef = AS.take([128, 16], F32)
    pAf = AS.take([128, 16], F32)
    pBf = AS.take([128, 16], F32)
    etf = AS.take([128, 32], F32)
    rtf = AS.take([128, 32], F32)
    D_cst = Dep("cst")
    D_s = Dep("sort")
    D_pos = Dep("pos")
    D_pW = Dep("pW")
    DMA("sp", [], [D_cst], "c_cst", dma(cstb[:], cstb_in), dma(cstf[:], cstf_in))
    thr = cstf[:, 0:16]
    iot = cstf[:, 16:48]
    pidx = cstf[:, 48:49]
    ones16 = cstf[:, 49:65]
    oh1v = oh1[:].rearrange("p b (g e) -> p b g e", e=4)
    oh2v = oh2[:].rearrange("p b (g e) -> p b g e", e=4)
    sst = []
    for gg in range(4):
        sst.append((lambda gg=gg: lambda e: e.tensor_tensor(out=oh1v[:, :, gg, :], in0=RT["mask1"][:],
                                                            in1=RT["gsel"][:, :, gg:gg + 1].to_broadcast([128, NB, 4]), op=ALU.mult))())
        sst.append((lambda gg=gg: lambda e: e.tensor_tensor(out=oh2v[:, :, gg, :], in0=RT["mask2"][:],
                                                            in1=RT["gsel"][:, :, gg:gg + 1].to_broadcast([128, NB, 4]), op=ALU.mult))())
    sst.append(lambda e: e.tensor_tensor(out=Mb[:].rearrange("p (b n) -> p b n", n=16), in0=oh1[:], in1=oh2[:], op=ALU.add))
    for i, f in enumerate(sst):
        P.op("dve", (lambda f=f: lambda e: [f(e)])(), R=[D_s] + ([D_r] if i == 0 else []), W=[D_s])
    pW = banks[1]
    OP("pe", [D_s, D_cst], [D_pW],
       mm(pW[:, 0:256], cstb[:, 0:128], Mb[:], True, True),
       mm(pW[:, 256:512], cstb[:, 128:256], Mb[:], True, True))
    wtf = wt[:].rearrange("p a b n -> p (a b n)")
    tot = wt[:, 1]
    sst = [lambda e: e.tensor_copy(out=wtf, in_=pW[:, :]),
           lambda e: e.memset(exb[:, 0, :], 0.0)]
    for b in range(1, NB):
        sst.append((lambda b=b: lambda e: e.tensor_tensor(out=exb[:, b, :], in0=exb[:, b - 1, :], in1=tot[:, b - 1, :], op=ALU.add))())
    sst += [
        lambda e: e.tensor_tensor(out=cnt[:], in0=exb[:, NB - 1, :], in1=tot[:, NB - 1, :], op=ALU.add),
        lambda e: e.tensor_tensor(out=cmpA[:], in0=cnt[:].unsqueeze(2).to_broadcast([128, 16, 16]),
                                  in1=thr.unsqueeze(1).to_broadcast([128, 16, 16]), op=ALU.is_gt),
        lambda e: e.reduce_sum(out=ntl[:], in_=cmpA[:], axis=AX.X),
        lambda e: e.tensor_tensor_scan(out=incl[:], data0=ones16, data1=ntl[:], initial=0.0, op0=ALU.mult, op1=ALU.add),
        lambda e: e.tensor_tensor(out=basef[:], in0=incl[:], in1=ntl[:], op=ALU.subtract),
        lambda e: e.tensor_scalar(out=basef[:], in0=basef[:], scalar1=float(TT), scalar2=None, op0=ALU.mult),
        lambda e: e.tensor_tensor(out=posf[:], in0=wt[:, 0], in1=exb[:], op=ALU.add),
        lambda e: e.tensor_tensor(out=posf[:], in0=posf[:], in1=basef[:].unsqueeze(1).to_broadcast([128, NB, 16]), op=ALU.add),
        lambda e: e.tensor_tensor(out=ptmp[:], in0=posf[:], in1=oh1[:], op=ALU.mult),
        lambda e: e.reduce_sum(out=pAf[:], in_=ptmp[:], axis=AX.X),
        lambda e: e.tensor_scalar(out=pAf[:], in0=pAf[:], scalar1=float(NSLOT - 1), scalar2=None, op0=ALU.min),
        lambda e: e.tensor_copy(out=posA_i[:], in_=pAf[:]),
        lambda e: e.tensor_tensor(out=ptmp[:], in0=posf[:], in1=oh2[:], op=ALU.mult),
        lambda e: e.reduce_sum(out=pBf[:], in_=ptmp[:], axis=AX.X),
        lambda e: e.tensor_scalar(out=pBf[:], in0=pBf[:], scalar1=float(NSLOT - 1), scalar2=None, op0=ALU.min),
        lambda e: e.tensor_copy(out=posB_i[:], in_=pBf[:]),
        lambda e: e.tensor_tensor(out=cmpB[:], in0=incl[:].unsqueeze(1).to_broadcast([128, 32, 16]),
                                  in1=iot.unsqueeze(2).to_broadcast([128, 32, 16]), op=ALU.is_le),
        lambda e: e.reduce_sum(out=etf[:], in_=cmpB[:], axis=AX.X),
        lambda e: e.reduce_sum(out=rtf[:], in_=cmpB[:].rearrange("p t (r j) -> p t r j", j=4)[:, :, :, 3], axis=AX.X),
        lambda e: e.tensor_scalar(out=etf[:], in0=etf[:], scalar1=512.0, scalar2=pidx, op0=ALU.mult, op1=ALU.add),
        lambda e: e.scalar_tensor_tensor(out=etf[:], in0=rtf[:], scalar=-1920.0, in1=etf[:], op0=ALU.mult, op1=ALU.add),
        lambda e: e.scalar_tensor_tensor(out=etf[:], in0=cmpB[:, :, 15], scalar=4096.0, in1=etf[:], op0=ALU.mult, op1=ALU.add),
        lambda e: e.tensor_copy(out=eidx_i[:], in_=etf[:]),
    ]
    for i, f in enumerate(sst):
        last = i == len(sst) - 1
        P.op("dve", (lambda f=f: lambda e: [f(e)])(), R=[D_s] + ([D_pW, D_cst] if i == 0 else []), W=[D_s] + ([D_pos] if last else []))

    if stage == 4:
        o_x1 = dout("o_x1a", [2048, 1024]); o_pa = dout("o_posA", [128, 16], I32); o_pb = dout("o_posB", [128, 16], I32)
        o_ei = dout("o_eidx", [128, 32], I32); o_m = dout("o_M", [128, 256], BF16)
        o_w1 = dout("o_w1", [128, 16]); o_w2 = dout("o_w2", [128, 16]); o_xn = dout("o_xn2", [128, 16 * 1024], BF16)
        DMA("sp", [D_X1d], [], "dbg", dma(o_x1, X1d))
        DMA("sp", [D_pos], [], "dbg", dma(o_pa, posA_i[:]), dma(o_pb, posB_i[:]), dma(o_ei, eidx_i[:]), dma(o_m, Mb[:]),
            dma(o_w1, RT["w1"][:].rearrange("p b o -> p (b o)")), dma(o_w2, RT["w2"][:].rearrange("p b o -> p (b o)")))
        DMA("sp", D_xn2b, [], "dbg", dma(o_xn, xn2_all[:].rearrange("p a b -> p (a b)")))
        return finish(nc, P, ["dbg"]), dbg

    P.fence()
    Ys = nc.dram_tensor("Ys", [NSLOT, 1024], F32).ap()
    D_Ys = Dep("Ys")
    D_sc = [Dep(f"sc{b}") for b in range(16)]
    NWB = 3
    A.top = m1
    wg = [A.take([128, 8, 512], BF16) for _ in range(NWB)]
    wu = [A.take([128, 8, 512], BF16) for _ in range(NWB)]
    wd = [A.take([128, 4, 1024], BF16) for _ in range(NWB)]
    xs_sb = [A.take([128, 2, 1024], BF16) for _ in range(2)]
    uTs = [A.take([128, 8, TT], BF16) for _ in range(2)]
    hT = [A.take([128, 4, TT], BF16) for _ in range(2)]
    sil = [A.take([128, TT], F32) for _ in range(2)]
    assert A.top <= mC, (A.top, mC)
    A.top = mKeep
    ys = [A.take([128, 2, 1024], F32) for _ in range(2)]
    g2_bc = A.take([128, 1024], F32)
    b2_bc = A.take([128, 1024], F32)
    NCB = 3
    lnsc5 = [mk_scratch() for _ in range(NCB)]
    nmr5 = [A.take([128, 1], F32) for _ in range(NCB)]
    D_bc2 = Dep("bc2")
    D_w = [[Dep(f"w{n}{i}") for i in range(NWB)] for n in "gud"]
    D_xs = [Dep("xs0"), Dep("xs1")]
    D_uTs = [Dep("uTs0"), Dep("uTs1")]
    D_hT = [Dep("hT0"), Dep("hT1")]
    D_sil = [Dep("sil0"), Dep("sil1")]
    D_pgu = [Dep("pgu0"), Dep("pgu1")]
    D_pd = [Dep(f"pd{i}") for i in range(4)]
    D_pT5 = [Dep("pT50"), Dep("pT51")]
    D_ys = [Dep("ys0"), Dep("ys1")]
    D_xa = [Dep(f"xa{i}") for i in range(NCB)]
    D_YA = [Dep(f"YA{i}") for i in range(NCB)]
    D_YB = [Dep(f"YB{i}") for i in range(NCB)]
    D_ot = [Dep(f"ot{i}") for i in range(NCB)]
    DMA("sp", [], [D_bc2], "c_bc2", dma(g2_bc[:], ln2_g.partition_broadcast(128)), dma(b2_bc[:], ln2_b.partition_broadcast(128)))

    def idma(out_, in_, out_off=None, in_off=None):
        return lambda e: e.indirect_dma_start(
            out=out_, out_offset=(bass.IndirectOffsetOnAxis(ap=out_off, axis=0) if out_off is not None else None),
            in_=in_, in_offset=(bass.IndirectOffsetOnAxis(ap=in_off, axis=0) if in_off is not None else None),
            )

    bnd = {}

    def set_bnd(e):
        bnd["r"] = nc.alloc_register(mybir.EngineType.Pool, "wbound")
        return e.reg_mov(bnd["r"], 16 * 128 - 1)
    OP("pool", [], [Dep("bnd")], set_bnd)

    def widma(out_, in_, off):
        return lambda e: e.indirect_dma_start(out=out_, out_offset=None, in_=in_, in_offset=bass.IndirectOffsetOnAxis(ap=off, axis=0),
                                              bounds_check=bnd["r"], oob_is_err=False)

    def load_wgu(t):
        s = t % NWB
        off = eidx_i[:, t:t + 1]
        DMA("pool", [D_pos, D_Wall[0]], [D_w[0][s]], f"ld_wg{s}", widma(wg[s][:].rearrange("p k f -> p (k f)"), Wb_all[0], off))
        DMA("pool", [D_pos, D_Wall[1]], [D_w[1][s]], f"ld_wu{s}", widma(wu[s][:].rearrange("p k f -> p (k f)"), Wb_all[1], off))

    def load_wd(t):
        s = t % NWB
        off = eidx_i[:, t:t + 1]
        DMA("pool", [D_pos, D_Wall[2]], [D_w[2][s]], f"ld_wd{s}", widma(wd[s][:].rearrange("p k f -> p (k f)"), Wb_all[2], off))

    def load_w(t):
        load_wgu(t)
        load_wd(t)

    if stage != 411:
        load_w(0)
    for b in range(16):
        if stage == 412:
            break
        DMA("pool", [D_pos, D_xn2b[b], D_Xs], [D_sc[b]], "sc_x",
            idma(Xs, xn2_all[:, b, :], out_off=posA_i[:, b:b + 1]),
            idma(Xs, xn2_all[:, b, :], out_off=posB_i[:, b:b + 1]))
    if stage != 411:
        load_w(1)
    load_w(2)
    if stage in (41, 410, 411, 412):
        o_xs = dout("o_xs", [NSLOT, 1024], BF16)
        DMA("sp", [D_Xs] + D_sc, [], "dbg", dma(o_xs, Xs))
        return finish(nc, P, ["dbg"]), dbg

    def load_x(t):
        s = t % 2
        DMA("sp", [D_Xs] + D_sc, [D_xs[s]], f"ld_xs{s}", dma(xs_sb[s][:], Xs[t * TT:(t + 1) * TT, :].rearrange("(i p) d -> p i d", p=128)))

    def tr(t):
        for i in range(2):
            tr_half(t % 2, i)

    def tr_half(s, i):
        pT = bank_bf(6 + i)
        csl = slice(i * 128, (i + 1) * 128)
        OP("pe", [D_xs[s], D_const], [D_pT5[i]],
           *[(lambda k=k: lambda e: e.transpose(out=pT[:, k, :], in_=xs_sb[s][:, i, k * 128:(k + 1) * 128], identity=ident_bf[:]))() for k in range(8)])
        OP("act", [D_pT5[i], D_mod], [], *[(lambda k=k: lambda e: e.activation(
            out=uTs[s][:, k, csl], in_=pT[:, k, :], func=AF.Identity, scale=sc_f1[:, k:k + 1], bias=sh_f[:, k:k + 1]))() for k in range(0, 8, 2)],
            Wp=[D_uTs[s]])
        OP("dve", [D_pT5[i], D_mod], [], *[(lambda k=k: lambda e: e.tensor_scalar(
            out=uTs[s][:, k, csl], in0=pT[:, k, :], scalar1=sc_f1[:, k:k + 1], scalar2=sh_f[:, k:k + 1], op0=ALU.mult, op1=ALU.add))()
            for k in range(1, 8, 2)], Wp=[D_uTs[s]])

    def gu(t):
        s = t % 2
        w = t % NWB
        for fc in range(4):
            q = fc % 2
            pgu = banks[q]
            fsl = slice(fc * 128, (fc + 1) * 128)
            OP("pe", [D_uTs[s], D_w[0][w], D_w[1][w]], [D_pgu[q]],
               *[mm(pgu[:, 0:TT], wg[w][:, k, fsl], uTs[s][:, k, :], k == 0, k == 7) for k in range(8)],
               *[mm(pgu[:, TT:2 * TT], wu[w][:, k, fsl], uTs[s][:, k, :], k == 0, k == 7) for k in range(8)])
            OP("act", [D_pgu[q]], [D_sil[q]], (lambda pgu=pgu, q=q: lambda e: e.activation(out=sil[q][:], in_=pgu[:, 0:TT], func=AF.Silu))())
            OP("dve", [D_sil[q], D_pgu[q]], [], (lambda pgu=pgu, q=q, fc=fc: lambda e: e.tensor_tensor(
                out=hT[s][:, fc, :], in0=sil[q][:], in1=pgu[:, TT:2 * TT], op=ALU.mult))(), Wp=[D_hT[s]])

    def down(t):
        s = t % 2
        w = t % NWB
        for i in range(2):
            for half in range(2):
                pi = i * 2 + half
                pd = banks[2 + pi]
                hc = slice(half * 512, (half + 1) * 512)
                OP("pe", [D_hT[s], D_w[2][w]], [D_pd[pi]],
                   *[mm(pd[:, :], hT[s][:, fc, i * 128:(i + 1) * 128], wd[w][:, fc, hc], fc == 0, fc == 3) for fc in range(4)])
                OP("dve", [D_pd[pi], D_gtf], [], (lambda pd=pd, i=i, hc=hc: lambda e: e.tensor_tensor(
                    out=ys[s][:, i, hc], in0=pd[:, :], in1=gtf_bc[:, hc], op=ALU.mult))(), Wp=[D_ys[s]])
        DMA("sp", [D_ys[s]], [], "st_ys", dma(Ys[t * TT:(t + 1) * TT, :].rearrange("(i p) d -> p i d", p=128), ys[s][:]), Wp=[D_Ys])

    NTR = NT_RUN[0]
    load_x(0)
    load_x(1)
    tr(0)
    if stage in (421, 422, 423):
        o_u = dout("o_uTs", [128, 8 * TT], BF16)
        DMA("sp", [D_uTs[0]], [], "dbg", dma(o_u, uTs[0][:].rearrange("p a b -> p (a b)")))
        if stage >= 422:
            o_wg = dout("o_wg", [128, 4096], BF16)
            o_wd = dout("o_wd", [128, 4096], BF16)
            DMA("sp", [D_w[0][0], D_w[2][0]], [], "dbg", dma(o_wg, wg[0][:].rearrange("p a b -> p (a b)")), dma(o_wd, wd[0][:].rearrange("p a b -> p (a b)")))
            gu(0)
            o_h = dout("o_hT", [128, 4 * TT], BF16)
            DMA("sp", [D_hT[0]], [], "dbg", dma(o_h, hT[0][:].rearrange("p a b -> p (a b)")))
        if stage >= 423:
            down(0)
            o_y = dout("o_ysb", [128, 2048])
            DMA("sp", [D_ys[0]], [], "dbg", dma(o_y, ys[0][:].rearrange("p a b -> p (a b)")))
        return finish(nc, P, ["dbg"]), dbg
    for t in range(NTR):
        gu(t)
        if t + 1 < NTR:
            tr(t + 1)
        if t + 2 < NTR:
            load_x(t + 2)
        if t + NWB < NTR:
            load_wgu(t + NWB)
        down(t)
        if t + NWB < NTR:
            load_wd(t + NWB)

    if stage == 42:
        o_ys = dout("o_ys", [NTR * TT, 1024])
        DMA("sp", [D_Ys], [], "dbg", *[dma(o_ys[t * TT:(t + 1) * TT, :], Ys[t * TT:(t + 1) * TT, :]) for t in range(NTR)])
        o_xs = dout("o_xs", [NSLOT, 1024], BF16)
        DMA("sp", [D_Xs] + D_sc, [], "dbg", dma(o_xs, Xs))
        o_pa = dout("o_posA", [128, 16], I32); o_pb = dout("o_posB", [128, 16], I32); o_ei = dout("o_eidx", [128, 32], I32)
        o_w1 = dout("o_w1", [128, 16]); o_w2 = dout("o_w2", [128, 16])
        DMA("sp", [D_pos], [], "dbg", dma(o_pa, posA_i[:]), dma(o_pb, posB_i[:]), dma(o_ei, eidx_i[:]),
            dma(o_w1, RT["w1"][:].rearrange("p b o -> p (b o)")), dma(o_w2, RT["w2"][:].rearrange("p b o -> p (b o)")))
        return finish(nc, P, ["dbg"]), dbg

    P.fence()
    A.top = m1
    xa = [A.take([128, 1024], F32) for _ in range(NCB)]
    YA = [A.take([128, 1024], F32) for _ in range(NCB)]
    YB = [A.take([128, 1024], F32) for _ in range(NCB)]
    ot = [A.take([128, 1024], F32) for _ in range(NCB)]

    def fin_gather(blk):
        s = blk % NCB
        DMA("sp", [D_X1d], [D_xa[s]], f"ld_xa{s}", dma(xa[s][:], X1d[blk * 128:(blk + 1) * 128, :]))
        DMA("pool", [D_Ys, D_pos], [D_YA[s]], f"ga{s}", idma(YA[s][:], Ys, in_off=posA_i[:, blk:blk + 1]))
        DMA("pool", [D_Ys, D_pos], [D_YB[s]], f"gb{s}", idma(YB[s][:], Ys, in_off=posB_i[:, blk:blk + 1]))

    def fin_compute(blk):
        s = blk % NCB
        st_, mv_, sd_, rstd_, D_s_ = lnsc5[s]
        OP("dve", [D_YA[s], D_xa[s], D_r], [D_xa[s]], lambda e: e.scalar_tensor_tensor(
            out=xa[s][:], in0=YA[s][:], scalar=RT["w1"][:, blk, :], in1=xa[s][:], op0=ALU.mult, op1=ALU.add))
        OP("dve", [D_YB[s], D_xa[s], D_r], [D_xa[s]], lambda e: e.scalar_tensor_tensor(
            out=xa[s][:], in0=YB[s][:], scalar=RT["w2"][:, blk, :], in1=xa[s][:], op0=ALU.mult, op1=ALU.add))
        OP("dve", [D_xa[s]], [D_s_],
           lambda e: e.bn_stats(out=st_[:, 0, :], in_=xa[s][:, 0:512]),
           lambda e: e.bn_stats(out=st_[:, 1, :], in_=xa[s][:, 512:1024]))
        OP("dve", [D_s_], [D_s_], lambda e: e.bn_aggr(out=mv_[:, :], in_=st_[:].rearrange("p a b -> p (a b)")))
        OP("act", [D_s_], [D_s_], lambda e: e.activation(out=sd_[:, :], in_=mv_[:, 1:2], func=AF.Sqrt, bias=1e-5, scale=1.0))
        OP("dve", [D_s_], [D_s_], lambda e: e.reciprocal(out=rstd_[:, :], in_=sd_[:, :]))
        OP("dve", [D_s_], [D_s_], lambda e: e.scalar_tensor_tensor(
            out=nmr5[s][:], in0=mv_[:, 0:1], scalar=-1.0, in1=rstd_[:, :], op0=ALU.mult, op1=ALU.mult))
        OP("act", [D_xa[s], D_s_], [D_ot[s]], lambda e: e.activation(
            out=ot[s][:], in_=xa[s][:], func=AF.Identity, scale=rstd_[:, 0:1], bias=nmr5[s][:, 0:1]))
        OP("pool", [D_ot[s], D_bc2], [D_ot[s]], lambda e: e.tensor_tensor(out=ot[s][:], in0=ot[s][:], in1=g2_bc[:], op=ALU.mult))
        OP("dve", [D_ot[s], D_bc2], [D_ot[s]], lambda e: e.tensor_tensor(out=ot[s][:], in0=ot[s][:], in1=b2_bc[:], op=ALU.add))
        DMA("sp", [D_ot[s]], [], "out", dma(out[blk * 128:(blk + 1) * 128, :], ot[s][:]))

    for blk in range(NCB - 1):
        fin_gather(blk)
    for blk in range(16):
        if blk + NCB - 1 < 16:
            fin_gather(blk + NCB - 1)
        fin_compute(blk)
    return finish(nc, P, ["out"]), dbg


def finish(nc, P, final_keys):
    from contextlib import ExitStack
    P.finalize()
    keys = sorted(set(P.dma_counts) | {"eng_" + e for e in COMPUTE})
    with ExitStack() as es:
        sems = {k: es.enter_context(nc.semaphore(k)) for k in keys}
        with nc.Block() as block:
            P.emit(block, sems, final_waits=sorted(P.dma_counts.items()))
    return nc


WEIGHT_KEYS = ["w_in", "b_f", "conv_w", "conv_b", "w_rg", "b_rg", "w_ig", "b_ig",
               "lru_lambda", "g_attn", "g_lru", "w_out", "ln1_g", "ln1_b", "w_grp", "b_grp", "w_exp", "b_exp",
               "ln2_g", "ln2_b"]


def make_in_maps(inputs):
    x = np.asarray(inputs["x"], dtype=np.float32)
    c = np.asarray(inputs["c"], dtype=np.float32)
    shared = {k: np.ascontiguousarray(np.asarray(inputs[k], dtype=np.float32)[0]) for k in WEIGHT_KEYS}
    w_e_gate = np.asarray(inputs["w_e_gate"], dtype=np.float32)[0]
    w_e_up = np.asarray(inputs["w_e_up"], dtype=np.float32)[0]
    w_e_down = np.asarray(inputs["w_e_down"], dtype=np.float32)[0]
    wpk = [np.ascontiguousarray(w_e_gate.reshape(16, 8, 128, 512).transpose(0, 2, 1, 3).reshape(16 * 128, 8 * 512)),
           np.ascontiguousarray(w_e_up.reshape(16, 8, 128, 512).transpose(0, 2, 1, 3).reshape(16 * 128, 8 * 512)),
           np.ascontiguousarray(w_e_down.reshape(16, 4, 128, 1024).transpose(0, 2, 1, 3).reshape(16 * 128, 4 * 1024))]
    kk = np.arange(128)[:, None]
    qq = np.arange(128)[None, :]
    tri = np.where(kk > qq, NEG, 0.0).astype(np.float32)
    cstb = np.zeros((128, 256), np.float32)
    cstb[:, 0:128] = (kk < qq)
    cstb[:, 128:256] = 1.0
    shared["cstb"] = cstb.astype(ml_dtypes.bfloat16)
    cstf = np.zeros((128, 80), np.float32)
    cstf[:, 0:16] = np.arange(16)[None, :] * TT
    cstf[:, 16:48] = np.arange(32)[None, :]
    cstf[:, 48] = np.arange(128)
    cstf[:, 49:65] = 1.0
    shared["cstf"] = cstf
    in_maps = []
    for core in range(8):
        b, j = divmod(core, 4)
        xb_ = x[b].reshape(16, 4, 128, 1024)
        x_loc = np.ascontiguousarray(xb_[:, j].reshape(2048, 1024))
        xh = np.zeros((16, 3, 1024), np.float32)
        for m in range(16):
            t0 = (4 * m + j) * 128
            if t0 >= 3:
                xh[m] = x[b, t0 - 3:t0]
        oh = np.zeros((128, 4), np.float32)
        oh[:, j] = 1.0
        hv = np.ones((128, 48), np.float32)
        if j == 0:
            hv[:, 0:3] = 0.0
        mask = np.zeros((128, 4, 128), np.float32)
        for r in range(4):
            if r == j:
                mask[:, r, :] = tri
            elif r > j:
                mask[:, r, :] = NEG
        wa_full = np.asarray(inputs["w_ada"], dtype=np.float32)[0]
        ba_full = np.asarray(inputs["b_ada"], dtype=np.float32)[0]
        m = {"w_ada_sh": np.ascontiguousarray(wa_full[:, j * 1536:(j + 1) * 1536]),
             "b_ada_sh": np.ascontiguousarray(ba_full[j * 1536:(j + 1) * 1536]),
             "x_loc": x_loc, "xh": xh.reshape(48, 1024), "c": np.ascontiguousarray(c[b]), "oh": oh, "hv": hv,
             "maskT": mask.astype(ml_dtypes.bfloat16)}
        for n_, w_ in zip(("wg_sh", "wu_sh", "wd_sh"), wpk):
            m[n_] = w_[j * 512:(j + 1) * 512]
        m.update(shared)
        in_maps.append(m)
    return in_maps


_NC_CACHE = {}


def kernel(**inputs):
    if "nc" not in _NC_CACHE:
        _NC_CACHE["nc"] = build()[0]
    nc = _NC_CACHE["nc"]
    in_maps = make_in_maps(inputs)
    res = run_bass_kernel_spmd(nc, in_maps, core_ids=list(range(8)))
    outp = np.zeros((2, 8192, 1024), np.float32)
    for core in range(8):
        b, j = divmod(core, 4)
        o = np.asarray(res.results[core]["out"]).reshape(16, 128, 1024)
        outp[b].reshape(16, 4, 128, 1024)[:, j] = o
    return outp
```

```python
import numpy as np
import ml_dtypes
import concourse.bass as bass
import concourse.mybir as mybir
from concourse.bass_utils import run_bass_kernel_spmd

F32 = mybir.dt.float32
BF16 = mybir.dt.bfloat16
U8 = mybir.dt.uint8
I32 = mybir.dt.int32
TT = 256
NT = 31
NSLOT = NT * TT
NT_RUN = [NT]
ALU = mybir.AluOpType
AF = mybir.ActivationFunctionType
AX = mybir.AxisListType

NEG = -30000.0
ALPHA = 2.0 ** 0.25


class Dep:
    __slots__ = ("name", "writers", "readers")

    def __init__(self, name=""):
        self.name = name
        self.writers = []
        self.readers = []


class _Op:
    __slots__ = ("idx", "eng", "fn", "deps", "dma", "key", "need_inc", "tok", "ndma", "inc")


COMPUTE = ("pe", "act", "dve", "pool")
STREAMS = ("pe", "act", "dve", "pool", "sp")


class Prog:
    def __init__(self):
        self.ops = []
        self.streams = {e: [] for e in STREAMS}
        self.dma_counts = {}
        self.last_compute = {e: None for e in COMPUTE}
        self.dma_open = []
        self.fence_set = set()
        self.keymap = {}
        self.free_keys = {True: [], False: []}
        self.key_sw = {}
        self.pinned = set()

    def op(self, eng, fn, R=(), W=(), Wp=(), dma=False, key=None, ndma=1, inc=16, nofence=False):
        o = _Op()
        o.inc = inc
        o.idx = len(self.ops)
        o.eng = eng
        o.fn = fn
        o.dma = dma
        o.key = key
        o.ndma = ndma
        o.need_inc = False
        o.tok = None
        deps = set(self.fence_set)
        raw = set()
        for r in R:
            deps.update(r.writers)
            raw.update(r.writers)
        for w in list(W) + list(Wp):
            deps.update(w.writers)
            deps.update(w.readers)
        keep = set()
        for d in deps:
            p = self.ops[d]
            if dma or p.dma or p.eng != eng:
                keep.add(d)
            elif eng != "pe":
                keep.add(d)
        o.deps = keep
        for d in keep:
            self.ops[d].need_inc = True
        for r in R:
            r.readers.append(o.idx)
        for w in W:
            w.writers = [o.idx]
            w.readers = []
        for w in Wp:
            w.writers = w.writers + [o.idx]
            w.readers = []
        if dma:
            pk = self.keymap.get(key)
            if pk is None:
                sw = (eng == "pool")
                fk = self.free_keys[sw]
                pk = fk.pop() if (fk and not nofence and inc == 16) else "k%d_%s" % (len(self.dma_counts), key)
                self.key_sw[pk] = sw
                self.keymap[key] = pk
            if nofence or inc != 16:
                self.pinned.add(pk)
            key = pk
            o.key = pk
            c = self.dma_counts.get(key, 0) + inc * ndma
            self.dma_counts[key] = c
            o.tok = (key, c)
            if not nofence:
                self.dma_open.append(o.idx)
        else:
            self.last_compute[eng] = o.idx
        self.ops.append(o)
        self.streams[eng].append(o)
        return o

    def fence(self):
        s = set(self.dma_open)
        for e in COMPUTE:
            if self.last_compute[e] is not None:
                s.add(self.last_compute[e])
        self.fence_set = s
        self.dma_open = []
        for k, pk in list(self.keymap.items()):
            if pk not in self.pinned:
                self.free_keys[self.key_sw[pk]].append(pk)
                del self.keymap[k]

    def finalize(self):
        cnt = {e: 0 for e in COMPUTE}
        for o in self.ops:
            if o.dma:
                continue
            if o.need_inc:
                cnt[o.eng] += 1
                o.tok = ("eng_" + o.eng, cnt[o.eng])
        self.eng_counts = cnt

    def emit(self, block, sems, final_waits=()):
        prog = self
        engs = {"pe": "tensor", "act": "scalar", "dve": "vector", "pool": "gpsimd", "sp": "sync"}

        def make(stream):
            def body(e):
                seen = {}
                for o in prog.streams[stream]:
                    need = {}
                    for d in o.deps:
                        k, v = prog.ops[d].tok
                        if need.get(k, 0) < v:
                            need[k] = v
                    for k, v in need.items():
                        if seen.get(k, 0) >= v:
                            continue
                        e.wait_ge(sems[k], v)
                        seen[k] = v
                    res = o.fn(e)
                    if not isinstance(res, (list, tuple)):
                        res = [res]
                    if o.dma:
                        assert len(res) == o.ndma, (len(res), o.ndma)
                        for ins in res:
                            ins.then_inc(sems[o.key], o.inc)
                    elif o.need_inc:
                        res[-1].then_inc(sems[o.tok[0]], 1)
                if stream == "sp":
                    for k, v in final_waits:
                        e.wait_ge(sems[k], v)
            return body

        for stream in STREAMS:
            getattr(block, engs[stream])(make(stream))


class Arena:
    def __init__(self, ap, size):
        self.ap = ap
        self.size = size
        self.top = 0

    def take(self, shape, dt, parts=None):
        esz = {F32: 4, BF16: 2, U8: 1, I32: 4}[dt]
        n = 1
        for s in shape[1:]:
            n *= s
        nb = n * esz
        off = (self.top + 63) // 64 * 64
        assert off + nb <= self.size, ("arena overflow", off, nb, self.size)
        self.top = off + nb
        v = self.ap[0:shape[0], off:off + nb]
        if dt != U8:
            v = v.bitcast(dt)
        if len(shape) == 3:
            v = v.rearrange("p (a b) -> p a b", a=shape[1])
        elif len(shape) == 4:
            v = v.rearrange("p (a b c) -> p a b c", a=shape[1], b=shape[2])
        return v


def build(stage=99):
    nc = bass.Bass("TRN2", target_bir_lowering=False)

    def din(name, shape, dt=F32):
        return nc.dram_tensor(name, shape, dt, kind="ExternalInput").ap()

    x_loc = din("x_loc", [2048, 1024])
    xh_in = din("xh", [48, 1024])
    c_in = din("c", [1024])
    oh_in = din("oh", [128, 4])
    hv_in = din("hv", [128, 48])
    mask_in = din("maskT", [128, 4, 128], BF16)
    w_ada_sh = din("w_ada_sh", [1024, 1536])
    b_ada_sh = din("b_ada_sh", [1536])
    w_in = din("w_in", [1024, 2568])
    b_f = din("b_f", [8])
    conv_w = din("conv_w", [4, 512])
    conv_b = din("conv_b", [512])
    w_rg = din("w_rg", [8, 64, 64])
    b_rg = din("b_rg", [512])
    w_ig = din("w_ig", [8, 64, 64])
    b_ig = din("b_ig", [512])
    lru_lambda = din("lru_lambda", [512])
    g_attn = din("g_attn", [512])
    g_lru = din("g_lru", [512])
    w_out = din("w_out", [1024, 1024])
    ln1_g = din("ln1_g", [1024])
    ln1_b = din("ln1_b", [1024])
    w_grp = din("w_grp", [1024, 4])
    b_grp = din("b_grp", [4])
    w_exp = din("w_exp", [1024, 16])
    b_exp = din("b_exp", [16])
    wsh_in = [din(n, [4 * 128, 4096]) for n in ("wg_sh", "wu_sh", "wd_sh")]
    Wb_loc = [nc.dram_tensor(f"Wb_loc{m}", [4 * 128, 4096], BF16).ap() for m in range(3)]
    Wb_all = [nc.dram_tensor(f"Wb_all{m}", [16 * 128, 4096], BF16).ap() for m in range(3)]
    cstb_in = din("cstb", [128, 256], BF16)
    cstf_in = din("cstf", [128, 80])
    ln2_g = din("ln2_g", [1024])
    ln2_b = din("ln2_b", [1024])
    out = nc.dram_tensor("out", [2048, 1024], F32, kind="ExternalOutput").ap()
    dbg = {}

    def dout(name, shape, dt=F32):
        dbg[name] = nc.dram_tensor(name, shape, dt, kind="ExternalOutput").ap()
        return dbg[name]

    Kd_loc = [nc.dram_tensor(f"Kd_loc{g}", [128, 2048], BF16).ap() for g in range(4)]
    Kd_all = [nc.dram_tensor(f"Kd_all{g}", [512, 2048], BF16).ap() for g in range(4)]
    Vd_loc = [nc.dram_tensor(f"Vd_loc{g}", [2048, 130], BF16).ap() for g in range(4)]
    Vd_all = [nc.dram_tensor(f"Vd_all{g}", [8192, 130], BF16).ap() for g in range(4)]
    Lf_loc = nc.dram_tensor("Lf_loc", [8, 2048], F32).ap()
    Lf_all = nc.dram_tensor("Lf_all", [32, 2048], F32).ap()
    Bd_loc = nc.dram_tensor("Bd_loc", [128, 128], F32).ap()
    Bd_all = nc.dram_tensor("Bd_all", [512, 128], F32).ap()
    Qd = nc.dram_tensor("Qd", [512, 2048], BF16).ap()
    Cq = nc.dram_tensor("Cq", [8, 3, 2048], BF16).ap()
    RG = [[0, 1, 2, 3], [4, 5, 6, 7]]

    P = Prog()
    ARENA_SZ = 204 * 1024
    arena_t = nc.alloc_sbuf_tensor("arena", [128, ARENA_SZ], U8)
    A = Arena(arena_t, ARENA_SZ)
    banks = [nc.alloc_psum_tensor(f"bank{i}", [128, 512], F32) for i in range(8)]

    def bank_bf(i):
        return banks[i][:, :].bitcast(BF16).rearrange("p (a b) -> p a b", a=8)

    def OP(eng, R, W, *fns, Wp=()):
        return P.op(eng, (lambda e: [f(e) for f in fns]), R=R, W=W, Wp=Wp)

    def CHAIN(eng, R, W, *fns):
        D_ch = Dep("chain")
        for i, f in enumerate(fns):
            P.op(eng, (lambda f=f: lambda e: [f(e)])(), R=list(R) + ([D_ch] if i else []), W=list(W) + [D_ch])

    def DMA(q, R, W, key, *fns, Wp=(), inc=16, nofence=False):
        return P.op(q, (lambda e: [f(e) for f in fns]), R=R, W=W, Wp=Wp, dma=True, key=key, ndma=len(fns), inc=inc, nofence=nofence)

    def dma(out_, in_, **kw):
        return lambda e: e.dma_start(out=out_, in_=in_, **kw)

    def dma_nc(out_, in_):
        return lambda e: e.dma_start(out=out_, in_=in_, allow_slow_non_contiguous=True)

    def mm(o, l, r, st, sp):
        return lambda e: e.matmul(o, lhsT=l, rhs=r, start=st, stop=sp)

    ident_bf = A.take([128, 128], BF16)
    ident_f = A.take([128, 128], F32)
    ones_f = A.take([128, 128], F32)
    modT = A.take([128, 32], F32)
    gtm_bc = A.take([128, 1024], F32)
    gtf_bc = A.take([128, 1024], F32)
    oh = A.take([128, 4], F32)
    D_const = Dep("const")
    D_mod = Dep("mod")
    D_gtm = Dep("gtm")
    D_gtf = Dep("gtf")
    D_oh = Dep("oh")

    def mk_ident(t):
        return [lambda e: e.memset(t, 1.0),
                lambda e: e.affine_select(out=t, in_=t, pattern=[[-1, 128]], compare_op=ALU.is_equal,
                                          fill=0.0, base=0, channel_multiplier=1)]
    CHAIN("pool", [], [D_const], *mk_ident(ident_bf[:]))
    CHAIN("pool", [], [Dep("idf")], *mk_ident(ident_f[:]))
    OP("pool", [], [Dep("onesf")], lambda e: e.memset(ones_f[:], 1.0))
    DMA("sp", [], [D_oh], "c_oh", dma(oh[:], oh_in))

    PERSIST = A.top

    ln_ctr = [0]

    def ln_apply(src, npart, D_src, dst, D_dst, scratch, eps=1e-5):
        st, mv, sd, rstd, D_s = scratch
        OP("dve", [D_src], [D_s],
           lambda e: e.bn_stats(out=st[0:npart, 0, :], in_=src[:, 0:512]),
           lambda e: e.bn_stats(out=st[0:npart, 1, :], in_=src[:, 512:1024]))
        OP("dve", [D_s], [D_s], lambda e: e.bn_aggr(out=mv[0:npart, :], in_=st[0:npart].rearrange("p a b -> p (a b)")))
        OP("act", [D_s], [D_s], lambda e: e.activation(out=sd[0:npart, :], in_=mv[0:npart, 1:2], func=AF.Sqrt, bias=eps, scale=1.0))
        OP("dve", [D_s], [D_s], lambda e: e.reciprocal(out=rstd[0:npart, :], in_=sd[0:npart, :]))
        OP("dve", [D_src, D_s], [D_dst],
           lambda e: e.tensor_scalar(out=dst, in0=src, scalar1=mv[0:npart, 0:1], scalar2=rstd[0:npart, 0:1],
                                     op0=ALU.subtract, op1=ALU.mult))

    def ln_apply_gen(src, npart, D_src, dst, D_dst, scratch, eps=1e-5, nmr=None):
        st, mv, sd, rstd, D_s = scratch
        OP("dve", [D_src], [D_s],
           lambda e: e.bn_stats(out=st[0:npart, 0, :], in_=src[:, 0:512]),
           lambda e: e.bn_stats(out=st[0:npart, 1, :], in_=src[:, 512:1024]))
        yield
        OP("dve", [D_s], [D_s], lambda e: e.bn_aggr(out=mv[0:npart, :], in_=st[0:npart].rearrange("p a b -> p (a b)")))
        yield
        OP("act", [D_s], [D_s], lambda e: e.activation(out=sd[0:npart, :], in_=mv[0:npart, 1:2], func=AF.Sqrt, bias=eps, scale=1.0))
        yield
        OP("dve", [D_s], [D_s], lambda e: e.reciprocal(out=rstd[0:npart, :], in_=sd[0:npart, :]))
        yield
        if nmr is None:
            OP("dve", [D_src, D_s], [D_dst],
               lambda e: e.tensor_scalar(out=dst, in0=src, scalar1=mv[0:npart, 0:1], scalar2=rstd[0:npart, 0:1],
                                         op0=ALU.subtract, op1=ALU.mult))
        else:
            OP("dve", [D_s], [D_s], lambda e: e.scalar_tensor_tensor(
                out=nmr[0:npart, :], in0=mv[0:npart, 0:1], scalar=-1.0, in1=rstd[0:npart, :], op0=ALU.mult, op1=ALU.mult))
            yield
            OP("act", [D_src, D_s], [D_dst], lambda e: e.activation(
                out=dst, in_=src, func=AF.Identity, scale=rstd[0:npart, 0:1], bias=nmr[0:npart, 0:1]))
        yield

    def mk_scratch_in(AA):
        return (AA.take([128, 2, 6], F32), AA.take([128, 2], F32), AA.take([128, 1], F32), AA.take([128, 1], F32), Dep("lnsc"))

    def mk_scratch():
        return (A.take([128, 2, 6], F32), A.take([128, 2], F32), A.take([128, 1], F32), A.take([128, 1], F32), Dep("lnsc"))

    m0 = A.top
    cT = A.take([128, 8], F32)
    csil = A.take([128, 8], F32)
    waf = A.take([128, 8, 1536], F32)
    brow = A.take([1, 1536], F32)
    mrow = A.take([1, 1536], F32)
    D_c = Dep("c")
    D_waf = Dep("waf")
    D_brow = Dep("brow")
    D_mrow = Dep("mrow")
    D_pm = [Dep(f"pm{i}") for i in range(3)]
    D_Md = Dep("Md")
    D_Ma = Dep("Ma")
    Md_loc = nc.dram_tensor("Md_loc", [1, 1536], F32).ap()
    Md_all = nc.dram_tensor("Md_all", [4, 1536], F32).ap()
    DMA("sp", [], [D_c], "c_c", dma_nc(cT[:], c_in.rearrange("(k p) -> p k", p=128)))
    DMA("sp", [], [D_brow], "c_bT", dma(brow[:], b_ada_sh.rearrange("(o n) -> o n", o=1)))
    wsh_v = w_ada_sh.rearrange("(k p) e -> p k e", p=128)
    DMA("sp", [], [D_waf], "ld_waf", *[dma(waf[:, 2 * i:2 * i + 2, :], wsh_v[:, 2 * i:2 * i + 2, :]) for i in range(4)])
    OP("act", [D_c], [D_c], lambda e: e.activation(out=csil[:], in_=cT[:], func=AF.Silu))
    for i in range(3):
        pb = banks[i]
        cs_ = slice(i * 512, (i + 1) * 512)
        OP("pe", [D_waf, D_c], [D_pm[i]], *[mm(pb[0:1, :], csil[:, k:k + 1], waf[:, k, cs_], k == 0, k == 7) for k in range(8)])
        OP("dve", [D_pm[i], D_brow], [], (lambda pb=pb, cs_=cs_: lambda e: e.tensor_tensor(
            out=mrow[:, cs_], in0=pb[0:1, :], in1=brow[:, cs_], op=ALU.add))(), Wp=[D_mrow])
    DMA("sp", [D_mrow], [D_Md], "st_md", dma(Md_loc, mrow[:]))
    DMA("pool", [D_Md], [D_Ma], "cc_md", lambda e: e.collective_compute(
        "AllGather", ALU.bypass, replica_groups=RG, ins=[Md_loc], outs=[Md_all]), inc=1)
    mflat = Md_all.rearrange("r c -> (r c)")
    DMA("sp", [D_Ma], [D_mod], "ld_mod",
        *[dma_nc(modT[:, gi * 8:(gi + 1) * 8], mflat[g * 1024:(g + 1) * 1024].rearrange("(k p) -> p k", p=128))
          for gi, g in enumerate([0, 1, 3, 4])])
    DMA("sp", [D_Ma], [D_gtm], "ld_gtm", dma(gtm_bc[:], mflat[2048:3072].partition_broadcast(128)))
    DMA("sp", [D_Ma], [D_gtf], "ld_gtf", dma(gtf_bc[:], mflat[5120:6144].partition_broadcast(128)))
    OP("dve", [D_mod], [D_mod],
       lambda e: e.tensor_scalar(out=modT[:, 8:16], in0=modT[:, 8:16], scalar1=1.0, scalar2=None, op0=ALU.add),
       lambda e: e.tensor_scalar(out=modT[:, 24:32], in0=modT[:, 24:32], scalar1=1.0, scalar2=None, op0=ALU.add))
    OP("pool", [D_gtm], [D_gtm], lambda e: e.tensor_scalar(out=gtm_bc[:], in0=gtm_bc[:], scalar1=1.0, scalar2=None, op0=ALU.add))
    OP("pool", [D_gtf], [D_gtf], lambda e: e.tensor_scalar(out=gtf_bc[:], in0=gtf_bc[:], scalar1=1.0, scalar2=None, op0=ALU.add))
    sh_m, sc_m1, sh_f, sc_f1 = modT[:, 0:8], modT[:, 8:16], modT[:, 16:24], modT[:, 24:32]
    P.fence()
    A.top = m0

    m1 = A.top
    xbh = A.take([128, 4, 16, 131], F32)
    ggb = A.take([128, 4, 2048], BF16)
    hv = A.take([128, 48], F32)
    lfT = A.take([8, 2048], F32)
    m1b = A.top
    uT = A.take([128, 8, 2048], BF16)
    uTh = A.take([128, 8, 48], BF16)
    win = A.take([128, 8, 2568], BF16)
    Vp = A.take([128, 16, 8, 65], BF16)
    qk_st = [A.take([128, 8, 512], BF16) for _ in range(2)]
    xt = [A.take([128, 1024], F32) for _ in range(2)]
    xn = [A.take([128, 1024], BF16) for _ in range(2)]
    lnsc = [mk_scratch() for _ in range(2)]
    gtmp = [A.take([128, 512], F32) for _ in range(3)]
    ftmp = A.take([8, 512], F32)
    bfT = A.take([8, 1], F32)
    D_uT = [Dep(f"uT{b}") for b in range(16)]
    D_uTh = Dep("uTh")
    D_xt = [Dep("xt0"), Dep("xt1")]
    D_xn = [Dep("xn0"), Dep("xn1")]
    D_pT = [Dep("pT0"), Dep("pT1")]
    D_win = [Dep(f"win{i}") for i in range(5)]
    D_xbh = Dep("xbh")
    D_ggb = Dep("ggb")
    D_lfT = Dep("lfT")
    D_Vp = Dep("Vp")
    D_hv = Dep("hv")
    D_bf = Dep("bf")
    win_v = w_in.rearrange("(k p) e -> p k e", p=128)
    colgrp = [(0, 512), (512, 1024), (1024, 1544), (1544, 2056), (2056, 2568)]
    for i, (a, b) in enumerate(colgrp):
        DMA("pool", [], [D_win[i]], f"win{i}", dma(win[:, :, a:b], win_v[:, :, a:b]))
    DMA("sp", [], [D_hv], "c_hv", dma(hv[:], hv_in))
    DMA("sp", [], [D_bf], "c_bf", dma_nc(bfT[:], b_f.rearrange("(h o) -> h o", o=1)))
    OP("pool", [], [D_Vp], lambda e: e.memset(Vp[:, :, :, 64:65], 1.0))
    wst = A.take([128, 4096], BF16)
    D_wst = Dep("wst")
    D_Wbl = [Dep(f"Wbl{m}") for m in range(3)]
    D_Wall = [Dep(f"Wall{m}") for m in range(3)]
    for m in range(3):
        for jj in range(4):
            DMA("pool", [], [D_wst], "ld_wst", dma(wst[:], wsh_in[m][jj * 128:(jj + 1) * 128, :]))
            DMA("pool", [D_wst], [], "st_wst", dma(Wb_loc[m][jj * 128:(jj + 1) * 128, :], wst[:]), Wp=[D_Wbl[m]])

    def ln_to_uT(blk, npart, src_dram, dstT, D_dst, s):
        DMA("sp", [], [D_xt[s]], f"xt{s}", dma(xt[s][0:npart, :], src_dram))
        ln_apply(xt[s][0:npart, :], npart, D_xt[s], xn[s][0:npart, :], D_xn[s], lnsc[s])
        pT = bank_bf(s)
        OP("pe", [D_xn[s], D_const], [D_pT[s]],
           *[(lambda k=k: lambda e: e.transpose(out=pT[:, k, 0:npart], in_=xn[s][0:npart, k * 128:(k + 1) * 128],
                                                identity=ident_bf[0:npart, 0:npart]))() for k in range(8)])
        OP("act", [D_pT[s], D_mod], [], *[(lambda k=k: lambda e: e.activation(
            out=dstT(k), in_=pT[:, k, 0:npart], func=AF.Identity, scale=sc_m1[:, k:k + 1], bias=sh_m[:, k:k + 1]))()
            for k in range(0, 8, 2)], Wp=[D_dst])
        OP("dve", [D_pT[s], D_mod], [], *[(lambda k=k: lambda e: e.tensor_scalar(
            out=dstT(k), in0=pT[:, k, 0:npart], scalar1=sc_m1[:, k:k + 1], scalar2=sh_m[:, k:k + 1],
            op0=ALU.mult, op1=ALU.add))() for k in range(1, 8, 2)], Wp=[D_dst])

    def ln_block(blk):
        ln_to_uT(blk, 128, x_loc[blk * 128:(blk + 1) * 128, :],
                 (lambda blk=blk: lambda k: uT[:, k, blk * 128:(blk + 1) * 128])(), D_uT[blk], blk % 2)

    ln_to_uT(-1, 48, xh_in, lambda k: uTh[:, k, :], D_uTh, 1)
    for blk in range(4):
        ln_block(blk)

    D_pA = [Dep(f"pA{i}") for i in range(4)]
    D_qk = [Dep("qk0"), Dep("qk1")]
    D_gt = [Dep(f"gtmp{i}") for i in range(3)]
    D_pF = Dep("pF")
    D_ft = Dep("ftmp")
    D_Kd = Dep("Kd")
    D_Qd = Dep("Qd")
    pa_i = [0]

    def next_pa():
        i = pa_i[0] % 4
        pa_i[0] += 1
        return banks[2 + i], D_pA[i]

    for cch in range(4):
        pb, D_p = next_pa()
        c0 = 1544 + cch * 128
        OP("pe", [D_uTh, D_win[3]], [D_p], *[mm(pb[:, 0:48], win[:, k, c0:c0 + 128], uTh[:, k, :], k == 0, k == 7) for k in range(8)])
        OP("dve", [D_p, D_hv], [], (lambda pb=pb, cch=cch: lambda e: e.tensor_tensor(
            out=xbh[:, cch, :, 0:3], in0=pb[:, 0:48].rearrange("p (m w) -> p m w", w=3),
            in1=hv[:].rearrange("p (m w) -> p m w", w=3), op=ALU.mult))(), Wp=[D_xbh])

    def do_tile(T):
        tcol = slice(T * 512, (T + 1) * 512)
        R_u = [D_uT[T * 4 + i] for i in range(4)]
        s = T % 2
        for oc in range(8):
            pb, D_p = next_pa()
            c0 = oc * 128
            OP("pe", R_u + [D_win[oc // 4]], [D_p], *[mm(pb[:, :], win[:, k, c0:c0 + 128], uT[:, k, tcol], k == 0, k == 7) for k in range(8)])
            if oc < 4:
                OP("act", [D_p], [], (lambda pb=pb, oc=oc: lambda e: e.activation(out=qk_st[s][:, oc, :], in_=pb[:, :], func=AF.Copy, scale=0.125))(), Wp=[D_qk[s]])
            else:
                OP("dve", [D_p], [], (lambda pb=pb, oc=oc: lambda e: e.tensor_copy(out=qk_st[s][:, oc, :], in_=pb[:, :]))(), Wp=[D_qk[s]])
            yield
        DMA("sp", [D_qk[s]], [], f"qk{s}",
            dma(Qd[:, tcol].rearrange("(c p) t -> p c t", p=128), qk_st[s][:, 0:4, :]),
            *[dma(Kd_loc[g][:, tcol], qk_st[s][:, 4 + g, :]) for g in range(4)], Wp=[D_Kd, D_Qd])
        for cch in range(4):
            pb, D_p = next_pa()
            c0 = 1544 + cch * 128
            OP("pe", R_u + [D_win[3]], [D_p], *[mm(pb[:, :], win[:, k, c0:c0 + 128], uT[:, k, tcol], k == 0, k == 7) for k in range(8)])
            OP("act", [D_p], [], (lambda pb=pb, cch=cch: lambda e: e.activation(
                out=xbh[:, cch, T * 4:(T + 1) * 4, 3:131], in_=pb[:, :].rearrange("p (m w) -> p m w", w=128), func=AF.Copy))(), Wp=[D_xbh])
            yield
        for cch in range(4):
            pb, D_p = next_pa()
            c0 = 2056 + cch * 128
            OP("pe", R_u + [D_win[4]], [D_p], *[mm(pb[:, :], win[:, k, c0:c0 + 128], uT[:, k, tcol], k == 0, k == 7) for k in range(8)])
            g0, g1, g2 = gtmp
            OP("act", [D_p], [D_gt[0]], (lambda pb=pb: lambda e: e.activation(out=g0[:], in_=pb[:, :], func=AF.Copy))())
            CHAIN("dve", [D_gt[0]], [D_gt[1]],
                  lambda e: e.tensor_tensor(out=g1[:], in0=g0[:], in1=g0[:], op=ALU.mult),
                  lambda e: e.tensor_scalar(out=g1[:], in0=g1[:], scalar1=0.044715, scalar2=1.0, op0=ALU.mult, op1=ALU.add),
                  lambda e: e.tensor_tensor(out=g1[:], in0=g1[:], in1=g0[:], op=ALU.mult))
            OP("act", [D_gt[1]], [D_gt[2]], lambda e: e.activation(out=g2[:], in_=g1[:], func=AF.Sigmoid, scale=1.5957691216057308))
            OP("dve", [D_gt[0], D_gt[2]], [], (lambda cch=cch: lambda e: e.tensor_tensor(
                out=ggb[:, cch, tcol], in0=g0[:], in1=g2[:], op=ALU.mult))(), Wp=[D_ggb])
            yield
        pf = banks[6]
        OP("pe", R_u + [D_win[2]], [D_pF], *[mm(pf[0:8, :], win[:, k, 1536:1544], uT[:, k, tcol], k == 0, k == 7) for k in range(8)])
        OP("dve", [D_pF, D_bf], [D_ft], lambda e: e.tensor_scalar(out=ftmp[:], in0=pf[0:8, :], scalar1=bfT[:, 0:1], scalar2=-1.0, op0=ALU.add, op1=ALU.mult))
        OP("act", [D_ft], [D_ft], lambda e: e.activation(out=ftmp[:], in_=ftmp[:], func=AF.Exp))
        OP("act", [D_ft], [], (lambda tcol=tcol: lambda e: e.activation(out=lfT[:, tcol], in_=ftmp[:], func=AF.Ln, bias=1.0, scale=1.0))(), Wp=[D_lfT])
        yield
        for i in range(4):
            blk = T * 4 + i
            pb, D_p = next_pa()
            OP("pe", [D_uT[blk], D_win[2]], [D_p], *[mm(pb[:, :], uT[:, k, blk * 128:(blk + 1) * 128], win[:, k, 1024:1536], k == 0, k == 7) for k in range(8)])
            if i % 2:
                OP("dve", [D_p], [], (lambda pb=pb, blk=blk: lambda e: e.tensor_copy(
                    out=Vp[:, blk, :, 0:64], in_=pb[:, :].rearrange("p (h d) -> p h d", d=64)))(), Wp=[D_Vp])
            else:
                OP("act", [D_p], [], (lambda pb=pb, blk=blk: lambda e: e.activation(
                    out=Vp[:, blk, :, 0:64], in_=pb[:, :].rearrange("p (h d) -> p h d", d=64), func=AF.Copy))(), Wp=[D_Vp])
            yield

    for T in range(4):
        nxt = list(range(4 * (T + 1), 4 * (T + 2))) if T < 3 else []
        for step, _ in enumerate(do_tile(T)):
            if nxt and step % 5 == 4:
                ln_block(nxt.pop(0))
        while nxt:
            ln_block(nxt.pop(0))

    D_Vd = Dep("Vd")
    D_Lfd = Dep("Lfd")
    DMA("sp", [D_Vp], [D_Vd], "st_v", *[dma(Vd_loc[g].rearrange("(m p) f -> p m f", p=128),
                                            Vp[:, :, 2 * g:2 * g + 2, :].rearrange("p m h d -> p m (h d)")) for g in range(4)])
    DMA("sp", [D_lfT], [D_Lfd], "st_lf", dma(Lf_loc, lfT[:]))
    D_Kall = [Dep(f"Kall{g}") for g in range(4)]
    D_Vall = [Dep(f"Vall{g}") for g in range(4)]
    D_Lfall = Dep("Lfall")

    def cc(in_ap, out_ap):
        return lambda e: e.collective_compute("AllGather", ALU.bypass, replica_groups=RG, ins=[in_ap], outs=[out_ap])
    if stage == 0:
        o_q = dout("o_q", [512, 2048], BF16)
        o_xbh = dout("o_xbh", [128, 4 * 16 * 131])
        DMA("sp", [D_Qd], [], "dbg", dma(o_q, Qd))
        DMA("sp", [D_xbh], [], "dbg", dma(o_xbh, xbh[:].rearrange("p a b c -> p (a b c)")))
        o_mod = dout("o_mod", [128, 32]); o_uT = dout("o_uT", [128, 8 * 2048], BF16); o_gtm = dout("o_gtm", [128, 1024])
        DMA("sp", [D_mod], [], "dbg", dma(o_mod, modT[:]))
        DMA("sp", [D_gtm], [], "dbg", dma(o_gtm, gtm_bc[:]))
        DMA("sp", D_uT, [], "dbg", dma(o_uT, uT[:].rearrange("p a b -> p (a b)")))
        return finish(nc, P, ["dbg"]), dbg
    def issue_kv_gathers(first):
        if first:
            DMA("pool", [D_Lfd], [D_Lfall], "cc_lf", cc(Lf_loc, Lf_all), inc=1, nofence=True)
        for g in ([0] if first else [1, 2, 3]):
            DMA("pool", [D_Kd], [D_Kall[g]], "cc_k", cc(Kd_loc[g], Kd_all[g]), inc=1, nofence=True)
            DMA("pool", [D_Vd], [D_Vall[g]], "cc_v", cc(Vd_loc[g], Vd_all[g]), inc=1, nofence=True)
    issue_kv_gathers(True)
    if stage == 1:
        issue_kv_gathers(False)

    if stage == 1:
        o_q = dout("o_q", [512, 2048], BF16)
        o_k = dout("o_kall", [512, 2048], BF16)
        o_v = dout("o_vall", [8192, 130], BF16)
        o_lf = dout("o_lfall", [32, 2048])
        o_xbh = dout("o_xbh", [128, 4 * 16 * 131])
        o_ggb = dout("o_ggb", [128, 4 * 2048], BF16)
        DMA("sp", [D_Qd], [], "dbg", dma(o_q, Qd))
        DMA("sp", [D_Kall[1]], [], "dbg", dma(o_k, Kd_all[1]))
        DMA("sp", [D_Vall[1]], [], "dbg", dma(o_v, Vd_all[1]))
        DMA("sp", [D_Lfall], [], "dbg", dma(o_lf, Lf_all))
        DMA("sp", [D_xbh], [], "dbg", dma(o_xbh, xbh[:].rearrange("p a b c -> p (a b c)")))
        DMA("sp", [D_ggb], [], "dbg", dma(o_ggb, ggb[:].rearrange("p a b -> p (a b)")))
        return finish(nc, P, ["dbg"]), dbg

    P.fence()
    A.top = m1b
    lruT = A.take([128, 4, 2048], BF16)
    D_lruT = Dep("lruT")
    mL = A.top
    cw = A.take([128, 4, 4], F32)
    cbv = A.take([128, 4], F32)
    brg = A.take([128, 4], F32)
    big = A.take([128, 4], F32)
    lam = A.take([128, 4], F32)
    cneg = A.take([128, 4], F32)
    cneg2 = A.take([128, 4], F32)
    wbd_f = A.take([128, 2, 4, 128], F32)
    wbd = A.take([128, 2, 4, 128], BF16)
    xc = A.take([128, 2048], F32)
    xcb = A.take([128, 2048], BF16)
    rr = A.take([128, 2048], F32)
    igt = A.take([128, 2048], F32)
    at2 = [A.take([128, 2048], F32) for _ in range(2)]
    t1 = A.take([128, 2048], F32)
    ut2 = [A.take([128, 2048], F32) for _ in range(2)]
    ht = A.take([128, 2048], F32)
    bl2 = [A.take([128, 32], F32) for _ in range(2)]
    ball2 = [A.take([128, 4, 32], F32) for _ in range(2)]
    D_at2 = [Dep("at0"), Dep("at1")]
    D_ut2 = [Dep("ut0"), Dep("ut1")]
    D_bl2 = [Dep("bl0"), Dep("bl1")]
    D_ball2 = [Dep("ball0"), Dep("ball1")]
    Ag = A.take([128, 64], F32)
    Hg = A.take([128, 64], F32)
    Sx = A.take([128, 68], F32)
    hin = A.take([128, 16], F32)
    tmp64 = A.take([128, 16, 4], F32)
    D_lc = Dep("lruconst")
    DMA("sp", [], [D_lc], "c_l",
        *[dma_nc(cw[:, :, w], conv_w[w].rearrange("(c p) -> p c", p=128)) for w in range(4)],
        dma_nc(cbv[:], conv_b.rearrange("(c p) -> p c", p=128)),
        dma_nc(brg[:], b_rg.rearrange("(c p) -> p c", p=128)),
        dma_nc(big[:], b_ig.rearrange("(c p) -> p c", p=128)),
        dma_nc(lam[:], lru_lambda.rearrange("(c p) -> p c", p=128)))
    OP("pool", [], [D_lc], lambda e: e.memset(wbd_f[:], 0.0))
    fl = []
    for gi, wsrc in enumerate((w_rg, w_ig)):
        for cch in range(4):
            for hh in range(2):
                fl.append(dma(wbd_f[hh * 64:(hh + 1) * 64, gi, cch, hh * 64:(hh + 1) * 64], wsrc[cch * 2 + hh]))
    DMA("sp", [D_lc], [D_lc], "c_l2", *fl)
    OP("dve", [D_lc], [D_lc], lambda e: e.tensor_copy(out=wbd[:], in_=wbd_f[:]))
    OP("act", [D_lc], [D_lc], lambda e: e.activation(out=cneg[:], in_=lam[:], func=AF.Exp, scale=-1.0))
    OP("act", [D_lc], [D_lc], lambda e: e.activation(out=cneg[:], in_=cneg[:], func=AF.Ln, bias=1.0, scale=1.0))
    CHAIN("dve", [D_lc], [D_lc],
          lambda e: e.tensor_scalar(out=cneg2[:], in0=cneg[:], scalar1=-16.0, scalar2=None, op0=ALU.mult),
          lambda e: e.tensor_scalar(out=cneg[:], in0=cneg[:], scalar1=-8.0, scalar2=None, op0=ALU.mult))
    D_l = {k: Dep(k) for k in "xc xcb rr ig at t1 ut ht bl ball ag sx hin pg0 pg1".split()}
    Bd_l = [nc.dram_tensor(f"Bd_l{c}", [128, 32], F32).ap() for c in range(4)]
    Bd_a = [nc.dram_tensor(f"Bd_a{c}", [512, 32], F32).ap() for c in range(4)]
    D_Bd = [Dep(f"Bd{c}") for c in range(4)]
    D_Ba = [Dep(f"Ba{c}") for c in range(4)]

    def lru_pass1(c):
        at, ut, bl = at2[c % 2], ut2[c % 2], bl2[c % 2]
        D_at, D_ut, D_bl = D_at2[c % 2], D_ut2[c % 2], D_bl2[c % 2]
        xv = xbh[:, c]
        xc3 = xc[:].rearrange("p (m w) -> p m w", w=128)
        CHAIN("dve", [D_xbh, D_lc], [D_l["xc"]],
           lambda e: e.tensor_scalar(out=xc3, in0=xv[:, :, 0:128], scalar1=cw[:, c, 0:1], scalar2=cbv[:, c:c + 1], op0=ALU.mult, op1=ALU.add),
           *[(lambda w=w: lambda e: e.scalar_tensor_tensor(out=xc3, in0=xv[:, :, w:w + 128], scalar=cw[:, c, w:w + 1], in1=xc3,
                                                           op0=ALU.mult, op1=ALU.add))() for w in (1, 2, 3)])
        OP("act", [D_l["xc"]], [D_l["xcb"]], lambda e: e.activation(out=xcb[:], in_=xc[:], func=AF.Copy))
        for T in range(4):
            tc_ = slice(T * 512, (T + 1) * 512)
            for gi, (dst, bia, nm) in enumerate(((rr, brg, "rr"), (igt, big, "ig"))):
                pb = banks[gi * 2 + T % 2]
                D_p = D_l["pg0"] if gi == 0 else D_l["pg1"]
                OP("pe", [D_l["xcb"], D_lc], [D_p], mm(pb[:, :], wbd[:, gi, c, :], xcb[:, tc_], True, True))
                OP("act", [D_p, D_lc], [], (lambda pb=pb, dst=dst, bia=bia, tc_=tc_: lambda e: e.activation(
                    out=dst[:, tc_], in_=pb[:, :], func=AF.Sigmoid, bias=bia[:, c:c + 1], scale=1.0))(), Wp=[D_l[nm]])
        OP("act", [D_l["rr"], D_lc], [D_at], lambda e: e.activation(out=at[:], in_=rr[:], func=AF.Exp, scale=cneg[:, c:c + 1]))
        OP("act", [D_l["rr"], D_lc], [D_l["t1"]], lambda e: e.activation(out=t1[:], in_=rr[:], func=AF.Exp, scale=cneg2[:, c:c + 1]))
        OP("act", [D_l["t1"]], [D_l["t1"]], lambda e: e.activation(out=t1[:], in_=t1[:], func=AF.Sqrt, bias=1.0, scale=-1.0))
        CHAIN("dve", [D_l["t1"], D_l["ig"], D_l["xc"]], [D_ut],
           lambda e: e.tensor_tensor(out=ut[:], in0=t1[:], in1=igt[:], op=ALU.mult),
           lambda e: e.tensor_tensor(out=ut[:], in0=ut[:], in1=xc[:], op=ALU.mult))
        OP("dve", [D_at, D_ut], [D_l["ht"]],
           *[(lambda m=m: lambda e: e.tensor_tensor_scan(out=ht[:, m * 128:(m + 1) * 128], data0=at[:, m * 128:(m + 1) * 128],
                                                         data1=ut[:, m * 128:(m + 1) * 128], initial=0.0, op0=ALU.mult, op1=ALU.add))()
             for m in range(16)])
        OP("dve", [D_l["ht"], D_l["rr"]], [D_bl],
           lambda e: e.tensor_copy(out=bl[:, 16:32], in_=ht[:].rearrange("p (m w) -> p m w", w=128)[:, :, 127]),
           lambda e: e.reduce_sum(out=bl[:, 0:16], in_=rr[:].rearrange("p (m w) -> p m w", w=128), axis=AX.X))
        OP("act", [D_bl, D_lc], [D_bl], lambda e: e.activation(out=bl[:, 0:16], in_=bl[:, 0:16], func=AF.Exp, scale=cneg[:, c:c + 1]))
        DMA("sp", [D_bl], [D_Bd[c]], "st_b", dma(Bd_l[c], bl[:]))
        DMA("pool", [D_Bd[c]], [D_Ba[c]], "cc_b", cc(Bd_l[c], Bd_a[c]), inc=1)

    def lru_pass2(c):
        at, ut, ball = at2[c % 2], ut2[c % 2], ball2[c % 2]
        D_at, D_ut, D_ball = D_at2[c % 2], D_ut2[c % 2], D_ball2[c % 2]
        DMA("sp", [D_Ba[c]], [D_ball], "ld_b", dma(ball[:], Bd_a[c].rearrange("(r p) f -> p r f", p=128)))
        OP("dve", [D_ball], [D_l["ag"]],
           lambda e: e.tensor_copy(out=Ag[:].rearrange("p (m r) -> p r m", r=4), in_=ball[:, :, 0:16]),
           lambda e: e.tensor_copy(out=Hg[:].rearrange("p (m r) -> p r m", r=4), in_=ball[:, :, 16:32]))
        OP("dve", [D_l["ag"]], [D_l["sx"]],
           lambda e: e.memset(Sx[:, 0:1], 0.0),
           lambda e: e.tensor_tensor_scan(out=Sx[:, 1:65], data0=Ag[:], data1=Hg[:], initial=0.0, op0=ALU.mult, op1=ALU.add))
        CHAIN("dve", [D_l["sx"], D_oh], [D_l["hin"]],
           lambda e: e.tensor_tensor(out=tmp64[:], in0=Sx[:, 0:64].rearrange("p (m r) -> p m r", r=4),
                                     in1=oh[:].unsqueeze(1).to_broadcast([128, 16, 4]), op=ALU.mult),
           lambda e: e.reduce_sum(out=hin[:], in_=tmp64[:], axis=AX.X))
        OP("dve", [D_at, D_ut, D_l["hin"]], [D_l["ht"]],
           *[(lambda m=m: lambda e: e.tensor_tensor_scan(out=ht[:, m * 128:(m + 1) * 128], data0=at[:, m * 128:(m + 1) * 128],
                                                         data1=ut[:, m * 128:(m + 1) * 128], initial=hin[:, m:m + 1], op0=ALU.mult, op1=ALU.add))()
             for m in range(16)])
        OP("dve", [D_l["ht"], D_ggb], [], lambda e: e.tensor_tensor(out=lruT[:, c, :], in0=ht[:], in1=ggb[:, c, :], op=ALU.mult), Wp=[D_lruT])

    lru_pass1(0)
    for c in range(4):
        if c + 1 < 4:
            lru_pass1(c + 1)
        lru_pass2(c)
    issue_kv_gathers(False)

    if stage == 2:
        o_l = dout("o_lru", [128, 4 * 2048], BF16)
        DMA("sp", [D_lruT], [], "dbg", dma(o_l, lruT[:].rearrange("p a b -> p (a b)")))
        return finish(nc, P, ["dbg"]), dbg

    P.fence()
    A.top = mL
    OnT = A.take([64, 8, 2048], BF16)
    D_OnT = Dep("OnT")
    mC = A.top
    Kh = [A.take([67, 8192], BF16) for _ in range(2)]
    Vh = [A.take([128, 64, 65], BF16) for _ in range(2)]
    Qh = [A.take([67, 2048], BF16) for _ in range(2)]
    pTt = [A.take([128, 512], BF16) for _ in range(4)]
    Osb = [A.take([65, 512], F32) for _ in range(2)]
    rs = [A.take([65, 512], F32) for _ in range(2)]
    D_Kh = [Dep("Kh0"), Dep("Kh1")]
    D_Vh = [Dep("Vh0"), Dep("Vh1")]
    D_Qh = [Dep("Qh0"), Dep("Qh1")]
    for s_ in range(2):
        OP("pool", [], [D_Kh[s_]], (lambda s_=s_: lambda e: e.memset(Kh[s_][64:67, :], 1.0))())

    def attn_load_kv(h, q="sp"):
        s_ = h % 2
        g = h // 2
        hh = h % 2
        Kv = Kh[s_][0:64, :].rearrange("d (m r p) -> d m r p", r=4, p=128)
        Vv = Vh[s_][:].rearrange("p (m r) d -> p m r d", r=4)
        DMA(q, [D_Kall[g]], [], f"ld_k{s_}",
            *[dma(Kv[:, :, r, :], Kd_all[g][r * 128 + hh * 64:r * 128 + hh * 64 + 64, :].rearrange("d (m p) -> d m p", p=128))
              for r in range(4)], Wp=[D_Kh[s_]])
        DMA(q, [D_Vall[g]], [D_Vh[s_]], f"ld_v{s_}",
            *[dma(Vv[:, :, r, :], Vd_all[g][r * 2048:(r + 1) * 2048, hh * 65:(hh + 1) * 65].rearrange("(m p) d -> p m d", p=128))
              for r in range(4)])
    attn_load_kv(0, "act")
    attn_load_kv(1, "act")
    mAttnEnd = A.top
    ATOP = Arena(arena_t, ARENA_SZ)
    ATOP.top = ARENA_SZ - 21 * 1024
    assert mAttnEnd <= ATOP.top, (mAttnEnd, ATOP.top)
    A2 = Arena(arena_t, m1b)
    A2.top = m1
    lfg = A2.take([8, 8192], F32)
    cnq = A2.take([8, 2048], F32)
    cnT = A2.take([128, 64, 8], F32)
    oh8 = A2.take([8, 4], F32)
    maskT = A2.take([128, 4, 128], BF16)
    r1 = ATOP.take([8, 2048], F32)
    aug = ATOP.take([8, 3, 2048], BF16)
    D_lfg = Dep("lfg")
    D_cnq = Dep("cnq")
    D_aug = Dep("aug")
    D_cnT = Dep("cnT")
    D_mask = Dep("mask")
    D_pc = Dep("pc")
    D_Cq = Dep("Cq")
    lfg4 = lfg[:].rearrange("h (m r p) -> h m r p", r=4, p=128)
    DMA("sp", [D_Lfall], [D_lfg], "ld_lf",
        *[dma(lfg4[:, :, r, :], Lf_all[r * 8:(r + 1) * 8, :].rearrange("h (m p) -> h m p", p=128)) for r in range(4)])
    DMA("sp", [], [D_mask], "c_mask", dma(maskT[:], mask_in), dma(oh8[:], oh_in[0:8, :]))
    OP("dve", [D_lfg, D_cnq], [D_lfg],
       lambda e: e.tensor_tensor_scan(out=lfg[:], data0=ones_f[0:8, 0:1].to_broadcast([8, 8192]), data1=lfg[:], initial=0.0,
                                      op0=ALU.mult, op1=ALU.add))
    cnq3 = cnq[:].rearrange("h (m p) -> h m p", p=128)
    CHAIN("dve", [D_lfg, D_mask], [D_cnq],
          lambda e: e.tensor_scalar(out=cnq3, in0=lfg4[:, :, 0, :], scalar1=oh8[:, 0:1], scalar2=None, op0=ALU.mult),
          *[(lambda r=r: lambda e: e.scalar_tensor_tensor(out=cnq3, in0=lfg4[:, :, r, :], scalar=oh8[:, r:r + 1], in1=cnq3,
                                                          op0=ALU.mult, op1=ALU.add))() for r in (1, 2, 3)])
    CHAIN("dve", [D_cnq], [D_aug],
          lambda e: e.tensor_scalar(out=aug[:, 0, :], in0=cnq[:], scalar1=-1.0, scalar2=None, op0=ALU.mult),
          lambda e: e.scalar_tensor_tensor(out=r1[:], in0=cnq[:], scalar=-1.0, in1=aug[:, 0, :], op0=ALU.mult, op1=ALU.subtract),
          lambda e: e.tensor_copy(out=aug[:, 1, :], in_=r1[:]),
          lambda e: e.tensor_tensor(out=r1[:], in0=r1[:], in1=aug[:, 1, :], op=ALU.subtract),
          lambda e: e.tensor_copy(out=aug[:, 2, :], in_=r1[:]))
    DMA("sp", [D_aug], [D_Cq], "st_cq", dma(Cq, aug[:]))
    pc = banks[7]
    OP("pe", [D_lfg, D_const], [D_pc],
       *[(lambda i=i: lambda e: e.transpose(out=pc[:, i * 8:(i + 1) * 8], in_=lfg[:, i * 128:(i + 1) * 128], identity=ident_f[0:8, 0:8]))()
         for i in range(64)])
    OP("act", [D_pc], [D_cnT], lambda e: e.activation(out=cnT[:].rearrange("p i h -> p (i h)"), in_=pc[:, :], func=AF.Copy))

    if stage == 25:
        o_cn = dout("o_cn", [8, 8192]); o_cnT = dout("o_cnT", [128, 512]); o_aug = dout("o_aug", [8, 3 * 2048], BF16)
        DMA("sp", [D_lfg], [], "dbg", dma(o_cn, lfg[:]))
        DMA("sp", [D_cnT], [], "dbg", dma(o_cnT, cnT[:].rearrange("p i h -> p (i h)")))
        DMA("sp", [D_aug], [], "dbg", dma(o_aug, aug[:].rearrange("h a t -> h (a t)")))
        return finish(nc, P, ["dbg"]), dbg

    D_pS = [Dep(f"pS{i}") for i in range(4)]
    D_pTt = [Dep(f"pTt{i}") for i in range(4)]
    D_pO = [Dep("pO0"), Dep("pO1")]
    D_Osb = [Dep("Osb0"), Dep("Osb1")]
    D_rs = [Dep("rs0"), Dep("rs1")]
    D_pB = Dep("pB")
    slot_ctr = [0]

    def attn_head(h):
        s_ = h % 2
        if h >= 2:
            attn_load_kv(h)
        DMA("sp", [D_Qd, D_Cq], [D_Qh[s_]], f"ld_q{s_}",
            dma(Qh[s_][0:64, :], Qd[h * 64:(h + 1) * 64, :]), dma(Qh[s_][64:67, :], Cq[h]))
        def do_qt(qt):
            items = [(kb, 0, None) for kb in range(16 * qt)]
            for dd in range(4):
                for r in range(4):
                    items.append(((4 * qt + dd) * 4 + r, 128 * dd, r))
            o = (h * 4 + qt) % 2
            pO = banks[4 + o]
            n = len(items)
            slots = {}

            def emit_S(idx):
                kb, c0, r = items[idx]
                sl = slot_ctr[0] % 4
                slot_ctr[0] += 1
                slots[idx] = sl
                pS = banks[sl]
                fns = [mm(pS[:, c0:512], Kh[s_][:, kb * 128:(kb + 1) * 128], Qh[s_][:, qt * 512 + c0:(qt + 1) * 512], True, True)]
                R = [D_Kh[s_], D_Qh[s_]]
                if r is not None:
                    fns.append(lambda e: e.matmul(pS[:, c0:c0 + 128], lhsT=ident_bf[:], rhs=maskT[:, r, :], start=False, stop=True,
                                                  skip_group_check=True))
                    R = R + [D_mask, D_const]
                OP("pe", R, [D_pS[sl]], *fns)

            def emit_rest(idx):
                kb, c0, r = items[idx]
                sl = slots[idx]
                pS = banks[sl]
                OP("act", [D_pS[sl], D_cnT], [D_pTt[sl]], lambda e: e.activation(
                    out=pTt[sl][:, c0:512], in_=pS[:, c0:512], func=AF.Exp, bias=cnT[:, kb, h:h + 1], scale=1.0))
                OP("pe", [D_pTt[sl], D_Vh[s_]], [], mm(pO[0:65, c0:512], Vh[s_][:, kb, :], pTt[sl][:, c0:512], idx == 0, idx == n - 1),
                   Wp=[D_pO[o]])

            emit_S(0)
            if n > 1:
                emit_S(1)
            for idx in range(n):
                if idx + 2 < n:
                    emit_S(idx + 2)
                emit_rest(idx)
            OP("act", [D_pO[o]], [D_Osb[o]], lambda e: e.activation(out=Osb[o][:, :], in_=pO[0:65, :], func=AF.Copy))
            OP("dve", [D_Osb[o]], [D_rs[o]], lambda e: e.reciprocal(out=rs[o][64:65, :], in_=Osb[o][64:65, :]))
            pB = banks[6]
            OP("pe", [D_rs[o], D_const], [D_pB], mm(pB[0:64, :], ones_f[64:65, 0:64], rs[o][64:65, :], True, True))
            OP("dve", [D_Osb[o], D_pB], [], lambda e: e.tensor_tensor(
                out=OnT[:, h, qt * 512:(qt + 1) * 512], in0=Osb[o][0:64, :], in1=pB[0:64, :], op=ALU.mult), Wp=[D_OnT])

        for qt in range(4):
            do_qt(qt)

    for m in range(3):
        for jj in range(4):
            DMA("pool", [D_Wbl[m]], [], "cc_w", cc(Wb_loc[m][jj * 128:(jj + 1) * 128, :], Wb_all[m][jj * 512:(jj + 1) * 512, :]),
                inc=1, nofence=True, Wp=[D_Wall[m]])
    for h in range(8):
        attn_head(h)

    if stage == 3:
        o_on = dout("o_on", [64, 8 * 2048], BF16)
        DMA("sp", [D_OnT], [], "dbg", dma(o_on, OnT[:].rearrange("p a b -> p (a b)")))
        return finish(nc, P, ["dbg"]), dbg

    P.fence()
    X1d = nc.dram_tensor("X1d", [2048, 1024], F32).ap()
    A2 = Arena(arena_t, m1b)
    A2.top = m1
    WoA = A2.take([64, 8, 1024], BF16)
    WoL = A2.take([128, 4, 1024], BF16)
    NW4 = 4
    xt4 = [A2.take([128, 1024], F32) for _ in range(NW4)]
    yv = [A2.take([128, 1024], F32) for _ in range(NW4)]
    A.top = mC
    xn2_all = A.take([128, 16, 1024], BF16)
    NB = 16
    RT = {k: A.take([128, NB, n], F32) for k, n in
          (("lg", 20), ("gmax", 1), ("ge", 4), ("gsum", 1), ("grpw", 1), ("gsel", 4), ("tmp", 16), ("sel", 4), ("m1", 1),
           ("mask1", 4), ("sel2", 4), ("m2", 1), ("mask2", 4), ("d21", 1), ("e2", 1), ("den", 1), ("w1", 1), ("w2", 1))}
    posA_i = A.take([128, 16], I32)
    posB_i = A.take([128, 16], I32)
    eidx_i = A.take([128, 32], I32)
    mKeep = A.top
    u2T = A.take([128, 8, 2048], BF16)
    mSort = A.top - 32 * 1024
    junk = [A.take([128, 2, 128], F32) for _ in range(NW4)]
    g1_bc = A.take([128, 1024], F32)
    b1_bc = A.take([128, 1024], F32)
    gaT = A.take([64, 8], F32)
    glT = A.take([128, 4], F32)
    wr = A.take([128, 8, 20], BF16)
    br_bc = A.take([128, 20], F32)
    lnsc4 = [mk_scratch_in(A) for _ in range(NW4)]
    ss = [A.take([128, 2], F32) for _ in range(NW4)]
    sd2 = [A.take([128, 2], F32) for _ in range(NW4)]
    rr2 = [A.take([128, 2], F32) for _ in range(NW4)]
    nmr4 = [A.take([128, 1], F32) for _ in range(NW4)]
    D_u2T = [Dep(f"u2T{b}") for b in range(16)]
    D_Wo = Dep("Wo")
    D_bc1 = Dep("bc1")
    D_wr = Dep("wr")
    D_xt4 = [Dep(f"xt4{i}") for i in range(NW4)]
    D_yv = [Dep(f"yv{i}") for i in range(NW4)]
    D_xn2b = [Dep(f"xn2b{b}") for b in range(16)]
    D_pG = Dep("pG")
    D_pYA = [Dep("pYA0"), Dep("pYA1")]
    D_pYL = [Dep("pYL0"), Dep("pYL1")]
    D_pT4 = Dep("pT4")
    D_pR = Dep("pR")
    D_ss = [Dep(f"ss{i}") for i in range(NW4)]
    D_rt = [Dep("rt0"), Dep("rt1")]
    D_X1d = Dep("X1d")
    DMA("pool", [], [D_Wo], "ld_wo",
        dma(WoA[:], w_out[0:512, :].rearrange("(h d) e -> d h e", d=64)),
        dma(WoL[:], w_out[512:1024, :].rearrange("(c p) e -> p c e", p=128)))
    DMA("sp", [], [D_Wo], "c_g",
        dma_nc(gaT[:], g_attn.rearrange("(h d) -> d h", d=64)), dma_nc(glT[:], g_lru.rearrange("(c p) -> p c", p=128)))
    DMA("sp", [], [D_bc1], "c_bc1", dma(g1_bc[:], ln1_g.partition_broadcast(128)), dma(b1_bc[:], ln1_b.partition_broadcast(128)),
        dma(br_bc[:, 0:4], b_grp.partition_broadcast(128)), dma(br_bc[:, 4:20], b_exp.partition_broadcast(128)))
    DMA("pool", [], [D_wr], "ld_wr",
        dma_nc(wr[:, :, 0:4], w_grp.rearrange("(k p) g -> p k g", p=128)), dma_nc(wr[:, :, 4:20], w_exp.rearrange("(k p) g -> p k g", p=128)))
    for h in range(8):
        OP("dve", [D_Wo, D_gtm], [D_Wo], (lambda h=h: lambda e: e.scalar_tensor_tensor(
            out=WoA[:, h, :], in0=WoA[:, h, :], scalar=gaT[:, h:h + 1], in1=gtm_bc[0:64, :], op0=ALU.mult, op1=ALU.mult))())
    for c in range(4):
        OP("dve", [D_Wo, D_gtm], [D_Wo], (lambda c=c: lambda e: e.scalar_tensor_tensor(
            out=WoL[:, c, :], in0=WoL[:, c, :], scalar=glT[:, c:c + 1], in1=gtm_bc[:], op0=ALU.mult, op1=ALU.mult))())
    D_X4 = [Dep(f"bX{i}") for i in range(NW4)]
    D_Y4 = [Dep(f"bY{i}") for i in range(NW4)]

    def post_block(blk):
        s = blk % NW4
        bc_ = slice(blk * 128, (blk + 1) * 128)
        DMA("sp", [], [D_xt4[s]], f"xt4{s}", dma(xt4[s][:], x_loc[blk * 128:(blk + 1) * 128, :]))
        pG = banks[s]
        OP("pe", [D_OnT, D_lruT], [D_X4[s]],
           *[mm(pG[:, 0:128], OnT[:, h, bc_], OnT[:, h, bc_], h == 0, h == 7) for h in range(8)],
           *[mm(pG[:, 128:256], lruT[:, c, bc_], lruT[:, c, bc_], c == 0, c == 3) for c in range(4)])
        yield
        OP("dve", [D_X4[s], D_const], [D_ss[s]], lambda e: e.tensor_tensor(
            out=junk[s][:], in0=pG[:, 0:256].rearrange("p (a b) -> p a b", a=2),
            in1=ident_f[:].unsqueeze(1).to_broadcast([128, 2, 128]), op=ALU.mult))
        yield
        OP("dve", [D_ss[s]], [D_ss[s]], lambda e: e.reduce_sum(out=ss[s][:], in_=junk[s][:], axis=AX.X))
        yield
        OP("act", [D_ss[s]], [D_ss[s]], lambda e: e.activation(out=sd2[s][:], in_=ss[s][:], func=AF.Sqrt, bias=1e-6, scale=1.0 / 512))
        yield
        OP("dve", [D_ss[s]], [D_ss[s]], lambda e: e.reciprocal(out=rr2[s][:], in_=sd2[s][:]))
        yield
        for half in range(2):
            hc = slice(half * 512, (half + 1) * 512)
            pYA = banks[4 + s]
            pYL = banks[4 + s]
            OP("pe", [D_OnT, D_Wo], [D_Y4[s]], *[mm(pYA[:, :], OnT[:, h, bc_], WoA[:, h, hc], h == 0, h == 7) for h in range(8)])
            yield
            OP("act", [D_Y4[s], D_ss[s]], [], (lambda pYA=pYA, hc=hc: lambda e: e.activation(
                out=yv[s][:, hc], in_=pYA[:, :], func=AF.Copy, scale=rr2[s][:, 0:1]))(), Wp=[D_yv[s]])
            yield
            OP("pe", [D_lruT, D_Wo], [D_Y4[s]], *[mm(pYL[:, :], lruT[:, c, bc_], WoL[:, c, hc], c == 0, c == 3) for c in range(4)])
            yield
            OP("dve", [D_Y4[s], D_ss[s], D_yv[s]], [], (lambda pYL=pYL, hc=hc: lambda e: e.scalar_tensor_tensor(
                out=yv[s][:, hc], in0=pYL[:, :], scalar=rr2[s][:, 1:2], in1=yv[s][:, hc], op0=ALU.mult, op1=ALU.add))(), Wp=[D_yv[s]])
            yield
        OP("dve", [D_yv[s], D_xt4[s]], [D_yv[s]], lambda e: e.scalar_tensor_tensor(
            out=yv[s][:], in0=xt4[s][:], scalar=ALPHA, in1=yv[s][:], op0=ALU.mult, op1=ALU.add))
        yield
        for _ in ln_apply_gen(yv[s][:], 128, D_yv[s], yv[s][:], D_yv[s], lnsc4[s], nmr=nmr4[s]):
            yield
        OP("pool", [D_yv[s], D_bc1], [D_yv[s]], lambda e: e.tensor_tensor(out=yv[s][:], in0=yv[s][:], in1=g1_bc[:], op=ALU.mult))
        yield
        OP("dve", [D_yv[s], D_bc1], [D_yv[s]], lambda e: e.tensor_tensor(out=yv[s][:], in0=yv[s][:], in1=b1_bc[:], op=ALU.add))
        yield
        for _ in ln_apply_gen(yv[s][:], 128, D_yv[s], xn2_all[:, blk, :], D_xn2b[blk], lnsc4[s], nmr=nmr4[s]):
            yield
        OP("act", [D_yv[s], D_xt4[s]], [D_xt4[s]], lambda e: e.activation(out=xt4[s][:], in_=yv[s][:], func=AF.Copy, scale=ALPHA))
        DMA("sp", [D_xt4[s]], [], "st_x1", dma(X1d[blk * 128:(blk + 1) * 128, :], xt4[s][:]), Wp=[D_X1d])
        pT = bank_bf(s)
        OP("pe", [D_xn2b[blk], D_const], [D_X4[s]],
           *[(lambda k=k: lambda e: e.transpose(out=pT[:, k, :], in_=xn2_all[:, blk, k * 128:(k + 1) * 128], identity=ident_bf[:]))() for k in range(8)])
        yield
        OP("act", [D_X4[s], D_mod], [], *[(lambda k=k: lambda e: e.activation(
            out=u2T[:, k, bc_], in_=pT[:, k, :], func=AF.Identity, scale=sc_f1[:, k:k + 1], bias=sh_f[:, k:k + 1]))() for k in range(0, 8, 2)],
            Wp=[D_u2T[blk]])
        OP("dve", [D_X4[s], D_mod], [], *[(lambda k=k: lambda e: e.tensor_scalar(
            out=u2T[:, k, bc_], in0=pT[:, k, :], scalar1=sc_f1[:, k:k + 1], scalar2=sh_f[:, k:k + 1], op0=ALU.mult, op1=ALU.add))()
            for k in range(1, 8, 2)], Wp=[D_u2T[blk]])
        yield

    def interleave(gens):
        gens = list(gens)
        while gens:
            for g_ in list(gens):
                try:
                    next(g_)
                except StopIteration:
                    gens.remove(g_)

    Xs = nc.dram_tensor("Xs", [NSLOT, 1024], BF16).ap()
    D_Xs = Dep("Xs")
    NZ = NSLOT // 128
    Xs_v = Xs.rearrange("(p a) d -> p a d", p=128)
    lru_rows = lruT[:].rearrange("p c (a d) -> p (c a) d", d=1024)
    DMA("sp", [D_lruT], [D_Xs], "z_xs", *[dma(Xs_v[:, a0:min(a0 + 8, NZ), :], lru_rows[:, 0:min(8, NZ - a0), :]) for a0 in range(0, NZ, 8)])
    for b0 in range(0, 16, NW4):
        interleave([post_block(b0 + i) for i in range(NW4)])

    pR = banks[0]
    D_pR = Dep("pR")
    OP("pe", D_u2T + [D_wr], [D_pR],
       *[mm(pR[:, blk * 20:(blk + 1) * 20], u2T[:, k, blk * 128:(blk + 1) * 128], wr[:, k, :], k == 0, k == 7)
         for blk in range(16) for k in range(8)])
    D_r = Dep("router")

    def bcl(ap, n):
        return ap.to_broadcast([128, NB, n])
    lg = RT["lg"]
    lgg = lg[:, :, 0:4]
    steps = [
        ("dve", lambda e: e.tensor_tensor(out=lg[:], in0=pR[:, 0:NB * 20].rearrange("p (b n) -> p b n", n=20),
                                          in1=br_bc[:].unsqueeze(1).to_broadcast([128, NB, 20]), op=ALU.add)),
        ("dve", lambda e: e.reduce_max(out=RT["gmax"][:].rearrange("p b o -> p (b o)"), in_=lgg, axis=AX.X)),
        ("dve", lambda e: e.tensor_tensor(out=RT["ge"][:], in0=lgg, in1=bcl(RT["gmax"][:], 4), op=ALU.subtract)),
        ("act", lambda e: e.activation(out=RT["ge"][:], in_=RT["ge"][:], func=AF.Exp)),
        ("dve", lambda e: e.reduce_sum(out=RT["gsum"][:].rearrange("p b o -> p (b o)"), in_=RT["ge"][:], axis=AX.X)),
        ("dve", lambda e: e.reciprocal(out=RT["grpw"][:], in_=RT["gsum"][:])),
        ("dve", lambda e: e.tensor_tensor(out=RT["gsel"][:], in0=lgg, in1=bcl(RT["gmax"][:], 4), op=ALU.is_equal)),
    ]
    tmpv = RT["tmp"][:].rearrange("p b (e g) -> p b e g", g=4)
    elv = lg[:, :, 4:20].rearrange("p b (g e) -> p b e g", e=4)
    for ee in range(4):
        steps.append(("dve", (lambda ee=ee: lambda e: e.tensor_tensor(out=tmpv[:, :, ee, :], in0=elv[:, :, ee, :], in1=RT["gsel"][:], op=ALU.mult))()))
    steps += [
        ("dve", lambda e: e.reduce_sum(out=RT["sel"][:].rearrange("p b e -> p (b e)"),
                                       in_=RT["tmp"][:].rearrange("p b (e g) -> p (b e) g", g=4), axis=AX.X)),
        ("dve", lambda e: e.reduce_max(out=RT["m1"][:].rearrange("p b o -> p (b o)"), in_=RT["sel"][:], axis=AX.X)),
        ("dve", lambda e: e.tensor_tensor(out=RT["mask1"][:], in0=RT["sel"][:], in1=bcl(RT["m1"][:], 4), op=ALU.is_equal)),
        ("dve", lambda e: e.scalar_tensor_tensor(out=RT["sel2"][:], in0=RT["mask1"][:], scalar=-1e30, in1=RT["sel"][:], op0=ALU.mult, op1=ALU.add)),
        ("dve", lambda e: e.reduce_max(out=RT["m2"][:].rearrange("p b o -> p (b o)"), in_=RT["sel2"][:], axis=AX.X)),
        ("dve", lambda e: e.tensor_tensor(out=RT["mask2"][:], in0=RT["sel2"][:], in1=bcl(RT["m2"][:], 4), op=ALU.is_equal)),
        ("dve", lambda e: e.tensor_tensor(out=RT["d21"][:], in0=RT["m2"][:], in1=RT["m1"][:], op=ALU.subtract)),
        ("act", lambda e: e.activation(out=RT["e2"][:], in_=RT["d21"][:], func=AF.Exp)),
        ("dve", lambda e: e.tensor_scalar(out=RT["den"][:], in0=RT["e2"][:], scalar1=1.0, scalar2=None, op0=ALU.add)),
        ("dve", lambda e: e.reciprocal(out=RT["den"][:], in_=RT["den"][:])),
        ("dve", lambda e: e.tensor_tensor(out=RT["w1"][:], in0=RT["den"][:], in1=RT["grpw"][:], op=ALU.mult)),
        ("dve", lambda e: e.tensor_tensor(out=RT["w2"][:], in0=RT["w1"][:], in1=RT["e2"][:], op=ALU.mult)),
    ]
    for i, (eng, f) in enumerate(steps):
        P.op(eng, (lambda f=f: lambda e: [f(e)])(), R=[D_r] + ([D_pR, D_bc1] if i == 0 else []), W=[D_r])

    P.fence()
    AS = Arena(arena_t, mSort + 32 * 1024)
    AS.top = mSort
    cstb = AS.take([128, 256], BF16)
    cstf = AS.take([128, 80], F32)
    oh1 = AS.take([128, NB, 16], F32)
    oh2 = AS.take([128, NB, 16], F32)
    Mb = AS.take([128, 256], BF16)
    wt = AS.take([128, 2, NB, 16], F32)
    exb = AS.take([128, NB, 16], F32)
    posf = AS.take([128, NB, 16], F32)
    ptmp = AS.take([128, NB, 16], F32)
    cmpA = AS.take([128, 16, 16], F32)
    cmpB = AS.take([128, 32, 16], F32)
    cnt = AS.take([128, 16], F32)
    ntl = AS.take([128, 16], F32)
    incl = AS.take([128, 16], F32)
    basef = AS.take([128, 16], F32)
    pAf = AS.take([128, 16], F32)
    pBf = AS.take([128, 16], F32)
    etf = AS.take([128, 32], F32)
    rtf = AS.take([128, 32], F32)
    D_cst = Dep("cst")
    D_s = Dep("sort")
    D_pos = Dep("pos")
    D_pW = Dep("pW")
    DMA("sp", [], [D_cst], "c_cst", dma(cstb[:], cstb_in), dma(cstf[:], cstf_in))
    thr = cstf[:, 0:16]
    iot = cstf[:, 16:48]
    pidx = cstf[:, 48:49]
    ones16 = cstf[:, 49:65]
    oh1v = oh1[:].rearrange("p b (g e) -> p b g e", e=4)
    oh2v = oh2[:].rearrange("p b (g e) -> p b g e", e=4)
    sst = []
    for gg in range(4):
        sst.append((lambda gg=gg: lambda e: e.tensor_tensor(out=oh1v[:, :, gg, :], in0=RT["mask1"][:],
                                                            in1=RT["gsel"][:, :, gg:gg + 1].to_broadcast([128, NB, 4]), op=ALU.mult))())
        sst.append((lambda gg=gg: lambda e: e.tensor_tensor(out=oh2v[:, :, gg, :], in0=RT["mask2"][:],
                                                            in1=RT["gsel"][:, :, gg:gg + 1].to_broadcast([128, NB, 4]), op=ALU.mult))())
    sst.append(lambda e: e.tensor_tensor(out=Mb[:].rearrange("p (b n) -> p b n", n=16), in0=oh1[:], in1=oh2[:], op=ALU.add))
    for i, f in enumerate(sst):
        P.op("dve", (lambda f=f: lambda e: [f(e)])(), R=[D_s] + ([D_r] if i == 0 else []), W=[D_s])
    pW = banks[1]
    OP("pe", [D_s, D_cst], [D_pW],
       mm(pW[:, 0:256], cstb[:, 0:128], Mb[:], True, True),
       mm(pW[:, 256:512], cstb[:, 128:256], Mb[:], True, True))
    wtf = wt[:].rearrange("p a b n -> p (a b n)")
    tot = wt[:, 1]
    sst = [lambda e: e.tensor_copy(out=wtf, in_=pW[:, :]),
           lambda e: e.memset(exb[:, 0, :], 0.0)]
    for b in range(1, NB):
        sst.append((lambda b=b: lambda e: e.tensor_tensor(out=exb[:, b, :], in0=exb[:, b - 1, :], in1=tot[:, b - 1, :], op=ALU.add))())
    sst += [
        lambda e: e.tensor_tensor(out=cnt[:], in0=exb[:, NB - 1, :], in1=tot[:, NB - 1, :], op=ALU.add),
        lambda e: e.tensor_tensor(out=cmpA[:], in0=cnt[:].unsqueeze(2).to_broadcast([128, 16, 16]),
                                  in1=thr.unsqueeze(1).to_broadcast([128, 16, 16]), op=ALU.is_gt),
        lambda e: e.reduce_sum(out=ntl[:], in_=cmpA[:], axis=AX.X),
        lambda e: e.tensor_tensor_scan(out=incl[:], data0=ones16, data1=ntl[:], initial=0.0, op0=ALU.mult, op1=ALU.add),
        lambda e: e.tensor_tensor(out=basef[:], in0=incl[:], in1=ntl[:], op=ALU.subtract),
        lambda e: e.tensor_scalar(out=basef[:], in0=basef[:], scalar1=float(TT), scalar2=None, op0=ALU.mult),
        lambda e: e.tensor_tensor(out=posf[:], in0=wt[:, 0], in1=exb[:], op=ALU.add),
        lambda e: e.tensor_tensor(out=posf[:], in0=posf[:], in1=basef[:].unsqueeze(1).to_broadcast([128, NB, 16]), op=ALU.add),
        lambda e: e.tensor_tensor(out=ptmp[:], in0=posf[:], in1=oh1[:], op=ALU.mult),
        lambda e: e.reduce_sum(out=pAf[:], in_=ptmp[:], axis=AX.X),
        lambda e: e.tensor_scalar(out=pAf[:], in0=pAf[:], scalar1=float(NSLOT - 1), scalar2=None, op0=ALU.min),
        lambda e: e.tensor_copy(out=posA_i[:], in_=pAf[:]),
        lambda e: e.tensor_tensor(out=ptmp[:], in0=posf[:], in1=oh2[:], op=ALU.mult),
        lambda e: e.reduce_sum(out=pBf[:], in_=ptmp[:], axis=AX.X),
        lambda e: e.tensor_scalar(out=pBf[:], in0=pBf[:], scalar1=float(NSLOT - 1), scalar2=None, op0=ALU.min),
        lambda e: e.tensor_copy(out=posB_i[:], in_=pBf[:]),
        lambda e: e.tensor_tensor(out=cmpB[:], in0=incl[:].unsqueeze(1).to_broadcast([128, 32, 16]),
                                  in1=iot.unsqueeze(2).to_broadcast([128, 32, 16]), op=ALU.is_le),
        lambda e: e.reduce_sum(out=etf[:], in_=cmpB[:], axis=AX.X),
        lambda e: e.reduce_sum(out=rtf[:], in_=cmpB[:].rearrange("p t (r j) -> p t r j", j=4)[:, :, :, 3], axis=AX.X),
        lambda e: e.tensor_scalar(out=etf[:], in0=etf[:], scalar1=512.0, scalar2=pidx, op0=ALU.mult, op1=ALU.add),
        lambda e: e.scalar_tensor_tensor(out=etf[:], in0=rtf[:], scalar=-1920.0, in1=etf[:], op0=ALU.mult, op1=ALU.add),
        lambda e: e.scalar_tensor_tensor(out=etf[:], in0=cmpB[:, :, 15], scalar=4096.0, in1=etf[:], op0=ALU.mult, op1=ALU.add),
        lambda e: e.tensor_copy(out=eidx_i[:], in_=etf[:]),
    ]
    for i, f in enumerate(sst):
        last = i == len(sst) - 1
        P.op("dve", (lambda f=f: lambda e: [f(e)])(), R=[D_s] + ([D_pW, D_cst] if i == 0 else []), W=[D_s] + ([D_pos] if last else []))

    if stage == 4:
        o_x1 = dout("o_x1a", [2048, 1024]); o_pa = dout("o_posA", [128, 16], I32); o_pb = dout("o_posB", [128, 16], I32)
        o_ei = dout("o_eidx", [128, 32], I32); o_m = dout("o_M", [128, 256], BF16)
        o_w1 = dout("o_w1", [128, 16]); o_w2 = dout("o_w2", [128, 16]); o_xn = dout("o_xn2", [128, 16 * 1024], BF16)
        DMA("sp", [D_X1d], [], "dbg", dma(o_x1, X1d))
        DMA("sp", [D_pos], [], "dbg", dma(o_pa, posA_i[:]), dma(o_pb, posB_i[:]), dma(o_ei, eidx_i[:]), dma(o_m, Mb[:]),
            dma(o_w1, RT["w1"][:].rearrange("p b o -> p (b o)")), dma(o_w2, RT["w2"][:].rearrange("p b o -> p (b o)")))
        DMA("sp", D_xn2b, [], "dbg", dma(o_xn, xn2_all[:].rearrange("p a b -> p (a b)")))
        return finish(nc, P, ["dbg"]), dbg

    P.fence()
    Ys = nc.dram_tensor("Ys", [NSLOT, 1024], F32).ap()
    D_Ys = Dep("Ys")
    D_sc = [Dep(f"sc{b}") for b in range(16)]
    NWB = 3
    A.top = m1
    wg = [A.take([128, 8, 512], BF16) for _ in range(NWB)]
    wu = [A.take([128, 8, 512], BF16) for _ in range(NWB)]
    wd = [A.take([128, 4, 1024], BF16) for _ in range(NWB)]
    xs_sb = [A.take([128, 2, 1024], BF16) for _ in range(2)]
    uTs = [A.take([128, 8, TT], BF16) for _ in range(2)]
    hT = [A.take([128, 4, TT], BF16) for _ in range(2)]
    sil = [A.take([128, TT], F32) for _ in range(2)]
    assert A.top <= mC, (A.top, mC)
    A.top = mKeep
    ys = [A.take([128, 2, 1024], F32) for _ in range(2)]
    g2_bc = A.take([128, 1024], F32)
    b2_bc = A.take([128, 1024], F32)
    NCB = 3
    lnsc5 = [mk_scratch() for _ in range(NCB)]
    nmr5 = [A.take([128, 1], F32) for _ in range(NCB)]
    D_bc2 = Dep("bc2")
    D_w = [[Dep(f"w{n}{i}") for i in range(NWB)] for n in "gud"]
    D_xs = [Dep("xs0"), Dep("xs1")]
    D_uTs = [Dep("uTs0"), Dep("uTs1")]
    D_hT = [Dep("hT0"), Dep("hT1")]
    D_sil = [Dep("sil0"), Dep("sil1")]
    D_pgu = [Dep("pgu0"), Dep("pgu1")]
    D_pd = [Dep(f"pd{i}") for i in range(4)]
    D_pT5 = [Dep("pT50"), Dep("pT51")]
    D_ys = [Dep("ys0"), Dep("ys1")]
    D_xa = [Dep(f"xa{i}") for i in range(NCB)]
    D_YA = [Dep(f"YA{i}") for i in range(NCB)]
    D_YB = [Dep(f"YB{i}") for i in range(NCB)]
    D_ot = [Dep(f"ot{i}") for i in range(NCB)]
    DMA("sp", [], [D_bc2], "c_bc2", dma(g2_bc[:], ln2_g.partition_broadcast(128)), dma(b2_bc[:], ln2_b.partition_broadcast(128)))

    def idma(out_, in_, out_off=None, in_off=None):
        return lambda e: e.indirect_dma_start(
            out=out_, out_offset=(bass.IndirectOffsetOnAxis(ap=out_off, axis=0) if out_off is not None else None),
            in_=in_, in_offset=(bass.IndirectOffsetOnAxis(ap=in_off, axis=0) if in_off is not None else None),
            )

    bnd = {}

    def set_bnd(e):
        bnd["r"] = nc.alloc_register(mybir.EngineType.Pool, "wbound")
        return e.reg_mov(bnd["r"], 16 * 128 - 1)
    OP("pool", [], [Dep("bnd")], set_bnd)

    def widma(out_, in_, off):
        return lambda e: e.indirect_dma_start(out=out_, out_offset=None, in_=in_, in_offset=bass.IndirectOffsetOnAxis(ap=off, axis=0),
                                              bounds_check=bnd["r"], oob_is_err=False)

    def load_wgu(t):
        s = t % NWB
        off = eidx_i[:, t:t + 1]
        DMA("pool", [D_pos, D_Wall[0]], [D_w[0][s]], f"ld_wg{s}", widma(wg[s][:].rearrange("p k f -> p (k f)"), Wb_all[0], off))
        DMA("pool", [D_pos, D_Wall[1]], [D_w[1][s]], f"ld_wu{s}", widma(wu[s][:].rearrange("p k f -> p (k f)"), Wb_all[1], off))

    def load_wd(t):
        s = t % NWB
        off = eidx_i[:, t:t + 1]
        DMA("pool", [D_pos, D_Wall[2]], [D_w[2][s]], f"ld_wd{s}", widma(wd[s][:].rearrange("p k f -> p (k f)"), Wb_all[2], off))

    def load_w(t):
        load_wgu(t)
        load_wd(t)

    if stage != 411:
        load_w(0)
    for b in range(16):
        if stage == 412:
            break
        DMA("pool", [D_pos, D_xn2b[b], D_Xs], [D_sc[b]], "sc_x",
            idma(Xs, xn2_all[:, b, :], out_off=posA_i[:, b:b + 1]),
            idma(Xs, xn2_all[:, b, :], out_off=posB_i[:, b:b + 1]))
    if stage != 411:
        load_w(1)
    load_w(2)
    if stage in (41, 410, 411, 412):
        o_xs = dout("o_xs", [NSLOT, 1024], BF16)
        DMA("sp", [D_Xs] + D_sc, [], "dbg", dma(o_xs, Xs))
        return finish(nc, P, ["dbg"]), dbg

    def load_x(t):
        s = t % 2
        DMA("sp", [D_Xs] + D_sc, [D_xs[s]], f"ld_xs{s}", dma(xs_sb[s][:], Xs[t * TT:(t + 1) * TT, :].rearrange("(i p) d -> p i d", p=128)))

    def tr(t):
        for i in range(2):
            tr_half(t % 2, i)

    def tr_half(s, i):
        pT = bank_bf(6 + i)
        csl = slice(i * 128, (i + 1) * 128)
        OP("pe", [D_xs[s], D_const], [D_pT5[i]],
           *[(lambda k=k: lambda e: e.transpose(out=pT[:, k, :], in_=xs_sb[s][:, i, k * 128:(k + 1) * 128], identity=ident_bf[:]))() for k in range(8)])
        OP("act", [D_pT5[i], D_mod], [], *[(lambda k=k: lambda e: e.activation(
            out=uTs[s][:, k, csl], in_=pT[:, k, :], func=AF.Identity, scale=sc_f1[:, k:k + 1], bias=sh_f[:, k:k + 1]))() for k in range(0, 8, 2)],
            Wp=[D_uTs[s]])
        OP("dve", [D_pT5[i], D_mod], [], *[(lambda k=k: lambda e: e.tensor_scalar(
            out=uTs[s][:, k, csl], in0=pT[:, k, :], scalar1=sc_f1[:, k:k + 1], scalar2=sh_f[:, k:k + 1], op0=ALU.mult, op1=ALU.add))()
            for k in range(1, 8, 2)], Wp=[D_uTs[s]])

    def gu(t):
        s = t % 2
        w = t % NWB
        for fc in range(4):
            q = fc % 2
            pgu = banks[q]
            fsl = slice(fc * 128, (fc + 1) * 128)
            OP("pe", [D_uTs[s], D_w[0][w], D_w[1][w]], [D_pgu[q]],
               *[mm(pgu[:, 0:TT], wg[w][:, k, fsl], uTs[s][:, k, :], k == 0, k == 7) for k in range(8)],
               *[mm(pgu[:, TT:2 * TT], wu[w][:, k, fsl], uTs[s][:, k, :], k == 0, k == 7) for k in range(8)])
            OP("act", [D_pgu[q]], [D_sil[q]], (lambda pgu=pgu, q=q: lambda e: e.activation(out=sil[q][:], in_=pgu[:, 0:TT], func=AF.Silu))())
            OP("dve", [D_sil[q], D_pgu[q]], [], (lambda pgu=pgu, q=q, fc=fc: lambda e: e.tensor_tensor(
                out=hT[s][:, fc, :], in0=sil[q][:], in1=pgu[:, TT:2 * TT], op=ALU.mult))(), Wp=[D_hT[s]])

    def down(t):
        s = t % 2
        w = t % NWB
        for i in range(2):
            for half in range(2):
                pi = i * 2 + half
                pd = banks[2 + pi]
                hc = slice(half * 512, (half + 1) * 512)
                OP("pe", [D_hT[s], D_w[2][w]], [D_pd[pi]],
                   *[mm(pd[:, :], hT[s][:, fc, i * 128:(i + 1) * 128], wd[w][:, fc, hc], fc == 0, fc == 3) for fc in range(4)])
                OP("dve", [D_pd[pi], D_gtf], [], (lambda pd=pd, i=i, hc=hc: lambda e: e.tensor_tensor(
                    out=ys[s][:, i, hc], in0=pd[:, :], in1=gtf_bc[:, hc], op=ALU.mult))(), Wp=[D_ys[s]])
        DMA("sp", [D_ys[s]], [], "st_ys", dma(Ys[t * TT:(t + 1) * TT, :].rearrange("(i p) d -> p i d", p=128), ys[s][:]), Wp=[D_Ys])

    NTR = NT_RUN[0]
    load_x(0)
    load_x(1)
    tr(0)
    if stage in (421, 422, 423):
        o_u = dout("o_uTs", [128, 8 * TT], BF16)
        DMA("sp", [D_uTs[0]], [], "dbg", dma(o_u, uTs[0][:].rearrange("p a b -> p (a b)")))
        if stage >= 422:
            o_wg = dout("o_wg", [128, 4096], BF16)
            o_wd = dout("o_wd", [128, 4096], BF16)
            DMA("sp", [D_w[0][0], D_w[2][0]], [], "dbg", dma(o_wg, wg[0][:].rearrange("p a b -> p (a b)")), dma(o_wd, wd[0][:].rearrange("p a b -> p (a b)")))
            gu(0)
            o_h = dout("o_hT", [128, 4 * TT], BF16)
            DMA("sp", [D_hT[0]], [], "dbg", dma(o_h, hT[0][:].rearrange("p a b -> p (a b)")))
        if stage >= 423:
            down(0)
            o_y = dout("o_ysb", [128, 2048])
            DMA("sp", [D_ys[0]], [], "dbg", dma(o_y, ys[0][:].rearrange("p a b -> p (a b)")))
        return finish(nc, P, ["dbg"]), dbg
    for t in range(NTR):
        gu(t)
        if t + 1 < NTR:
            tr(t + 1)
        if t + 2 < NTR:
            load_x(t + 2)
        if t + NWB < NTR:
            load_wgu(t + NWB)
        down(t)
        if t + NWB < NTR:
            load_wd(t + NWB)

    if stage == 42:
        o_ys = dout("o_ys", [NTR * TT, 1024])
        DMA("sp", [D_Ys], [], "dbg", *[dma(o_ys[t * TT:(t + 1) * TT, :], Ys[t * TT:(t + 1) * TT, :]) for t in range(NTR)])
        o_xs = dout("o_xs", [NSLOT, 1024], BF16)
        DMA("sp", [D_Xs] + D_sc, [], "dbg", dma(o_xs, Xs))
        o_pa = dout("o_posA", [128, 16], I32); o_pb = dout("o_posB", [128, 16], I32); o_ei = dout("o_eidx", [128, 32], I32)
        o_w1 = dout("o_w1", [128, 16]); o_w2 = dout("o_w2", [128, 16])
        DMA("sp", [D_pos], [], "dbg", dma(o_pa, posA_i[:]), dma(o_pb, posB_i[:]), dma(o_ei, eidx_i[:]),
            dma(o_w1, RT["w1"][:].rearrange("p b o -> p (b o)")), dma(o_w2, RT["w2"][:].rearrange("p b o -> p (b o)")))
        return finish(nc, P, ["dbg"]), dbg

    P.fence()
    A.top = m1
    xa = [A.take([128, 1024], F32) for _ in range(NCB)]
    YA = [A.take([128, 1024], F32) for _ in range(NCB)]
    YB = [A.take([128, 1024], F32) for _ in range(NCB)]
    ot = [A.take([128, 1024], F32) for _ in range(NCB)]

    def fin_gather(blk):
        s = blk % NCB
        DMA("sp", [D_X1d], [D_xa[s]], f"ld_xa{s}", dma(xa[s][:], X1d[blk * 128:(blk + 1) * 128, :]))
        DMA("pool", [D_Ys, D_pos], [D_YA[s]], f"ga{s}", idma(YA[s][:], Ys, in_off=posA_i[:, blk:blk + 1]))
        DMA("pool", [D_Ys, D_pos], [D_YB[s]], f"gb{s}", idma(YB[s][:], Ys, in_off=posB_i[:, blk:blk + 1]))

    def fin_compute(blk):
        s = blk % NCB
        st_, mv_, sd_, rstd_, D_s_ = lnsc5[s]
        OP("dve", [D_YA[s], D_xa[s], D_r], [D_xa[s]], lambda e: e.scalar_tensor_tensor(
            out=xa[s][:], in0=YA[s][:], scalar=RT["w1"][:, blk, :], in1=xa[s][:], op0=ALU.mult, op1=ALU.add))
        OP("dve", [D_YB[s], D_xa[s], D_r], [D_xa[s]], lambda e: e.scalar_tensor_tensor(
            out=xa[s][:], in0=YB[s][:], scalar=RT["w2"][:, blk, :], in1=xa[s][:], op0=ALU.mult, op1=ALU.add))
        OP("dve", [D_xa[s]], [D_s_],
           lambda e: e.bn_stats(out=st_[:, 0, :], in_=xa[s][:, 0:512]),
           lambda e: e.bn_stats(out=st_[:, 1, :], in_=xa[s][:, 512:1024]))
        OP("dve", [D_s_], [D_s_], lambda e: e.bn_aggr(out=mv_[:, :], in_=st_[:].rearrange("p a b -> p (a b)")))
        OP("act", [D_s_], [D_s_], lambda e: e.activation(out=sd_[:, :], in_=mv_[:, 1:2], func=AF.Sqrt, bias=1e-5, scale=1.0))
        OP("dve", [D_s_], [D_s_], lambda e: e.reciprocal(out=rstd_[:, :], in_=sd_[:, :]))
        OP("dve", [D_s_], [D_s_], lambda e: e.scalar_tensor_tensor(
            out=nmr5[s][:], in0=mv_[:, 0:1], scalar=-1.0, in1=rstd_[:, :], op0=ALU.mult, op1=ALU.mult))
        OP("act", [D_xa[s], D_s_], [D_ot[s]], lambda e: e.activation(
            out=ot[s][:], in_=xa[s][:], func=AF.Identity, scale=rstd_[:, 0:1], bias=nmr5[s][:, 0:1]))
        OP("dve", [D_ot[s], D_bc2], [D_ot[s]], lambda e: e.tensor_tensor(out=ot[s][:], in0=ot[s][:], in1=g2_bc[:], op=ALU.mult))
        OP("dve", [D_ot[s], D_bc2], [D_ot[s]], lambda e: e.tensor_tensor(out=ot[s][:], in0=ot[s][:], in1=b2_bc[:], op=ALU.add))
        DMA("sp", [D_ot[s]], [], "out", dma(out[blk * 128:(blk + 1) * 128, :], ot[s][:]))

    for blk in range(NCB - 1):
        fin_gather(blk)
    for blk in range(16):
        if blk + NCB - 1 < 16:
            fin_gather(blk + NCB - 1)
        fin_compute(blk)
    return finish(nc, P, ["out"]), dbg


def finish(nc, P, final_keys):
    from contextlib import ExitStack
    P.finalize()
    keys = sorted(set(P.dma_counts) | {"eng_" + e for e in COMPUTE})
    with ExitStack() as es:
        sems = {k: es.enter_context(nc.semaphore(k)) for k in keys}
        with nc.Block() as block:
            P.emit(block, sems, final_waits=sorted(P.dma_counts.items()))
    return nc


WEIGHT_KEYS = ["w_in", "b_f", "conv_w", "conv_b", "w_rg", "b_rg", "w_ig", "b_ig",
               "lru_lambda", "g_attn", "g_lru", "w_out", "ln1_g", "ln1_b", "w_grp", "b_grp", "w_exp", "b_exp",
               "ln2_g", "ln2_b"]


def make_in_maps(inputs):
    x = np.asarray(inputs["x"], dtype=np.float32)
    c = np.asarray(inputs["c"], dtype=np.float32)
    shared = {k: np.ascontiguousarray(np.asarray(inputs[k], dtype=np.float32)[0]) for k in WEIGHT_KEYS}
    w_e_gate = np.asarray(inputs["w_e_gate"], dtype=np.float32)[0]
    w_e_up = np.asarray(inputs["w_e_up"], dtype=np.float32)[0]
    w_e_down = np.asarray(inputs["w_e_down"], dtype=np.float32)[0]
    wpk = [np.ascontiguousarray(w_e_gate.reshape(16, 8, 128, 512).transpose(0, 2, 1, 3).reshape(16 * 128, 8 * 512)),
           np.ascontiguousarray(w_e_up.reshape(16, 8, 128, 512).transpose(0, 2, 1, 3).reshape(16 * 128, 8 * 512)),
           np.ascontiguousarray(w_e_down.reshape(16, 4, 128, 1024).transpose(0, 2, 1, 3).reshape(16 * 128, 4 * 1024))]
    kk = np.arange(128)[:, None]
    qq = np.arange(128)[None, :]
    tri = np.where(kk > qq, NEG, 0.0).astype(np.float32)
    cstb = np.zeros((128, 256), np.float32)
    cstb[:, 0:128] = (kk < qq)
    cstb[:, 128:256] = 1.0
    shared["cstb"] = cstb.astype(ml_dtypes.bfloat16)
    cstf = np.zeros((128, 80), np.float32)
    cstf[:, 0:16] = np.arange(16)[None, :] * TT
    cstf[:, 16:48] = np.arange(32)[None, :]
    cstf[:, 48] = np.arange(128)
    cstf[:, 49:65] = 1.0
    shared["cstf"] = cstf
    in_maps = []
    for core in range(8):
        b, j = divmod(core, 4)
        xb_ = x[b].reshape(16, 4, 128, 1024)
        x_loc = np.ascontiguousarray(xb_[:, j].reshape(2048, 1024))
        xh = np.zeros((16, 3, 1024), np.float32)
        for m in range(16):
            t0 = (4 * m + j) * 128
            if t0 >= 3:
                xh[m] = x[b, t0 - 3:t0]
        oh = np.zeros((128, 4), np.float32)
        oh[:, j] = 1.0
        hv = np.ones((128, 48), np.float32)
        if j == 0:
            hv[:, 0:3] = 0.0
        mask = np.zeros((128, 4, 128), np.float32)
        for r in range(4):
            if r == j:
                mask[:, r, :] = tri
            elif r > j:
                mask[:, r, :] = NEG
        wa_full = np.asarray(inputs["w_ada"], dtype=np.float32)[0]
        ba_full = np.asarray(inputs["b_ada"], dtype=np.float32)[0]
        m = {"w_ada_sh": np.ascontiguousarray(wa_full[:, j * 1536:(j + 1) * 1536]),
             "b_ada_sh": np.ascontiguousarray(ba_full[j * 1536:(j + 1) * 1536]),
             "x_loc": x_loc, "xh": xh.reshape(48, 1024), "c": np.ascontiguousarray(c[b]), "oh": oh, "hv": hv,
             "maskT": mask.astype(ml_dtypes.bfloat16)}
        for n_, w_ in zip(("wg_sh", "wu_sh", "wd_sh"), wpk):
            m[n_] = w_[j * 512:(j + 1) * 512]
        m.update(shared)
        in_maps.append(m)
    return in_maps


_NC_CACHE = {}


def kernel(**inputs):
    if "nc" not in _NC_CACHE:
        _NC_CACHE["nc"] = build()[0]
    nc = _NC_CACHE["nc"]
    in_maps = make_in_maps(inputs)
    res = run_bass_kernel_spmd(nc, in_maps, core_ids=list(range(8)))
    outp = np.zeros((2, 8192, 1024), np.float32)
    for core in range(8):
        b, j = divmod(core, 4)
        o = np.asarray(res.results[core]["out"]).reshape(16, 128, 1024)
        outp[b].reshape(16, 4, 128, 1024)[:, j] = o
    return outp
```
